# Optimizing a Trainium2 kernel written in Bass

```python
import jax
import jax.numpy as jnp
from jax import lax
import numpy as np


D_MODEL = 1024
BATCH = 8
SEQ = 8192
DEPTH = 1

CTX_LEN = 256
GRID_W = 64
EPS = 1e-6

POOL_WINDOWS = (2, 4, 8, 16)
POOL_GROUPS = len(POOL_WINDOWS)
POOL_WIDTH = D_MODEL // 2
POOL_GC = POOL_WIDTH // POOL_GROUPS

M_HEADS = 4
M_HEAD_DIM = D_MODEL // 8
M_WIDTH = M_HEADS * M_HEAD_DIM
CONV_W = 3
CHUNK = 128
F_BIAS_INIT = 3.0

OFF_POOL = 0
OFF_Q = OFF_POOL + POOL_WIDTH
OFF_K = OFF_Q + M_WIDTH
OFF_V = OFF_K + M_WIDTH
OFF_O = OFF_V + M_WIDTH
OFF_GATE = OFF_O + M_WIDTH
N_GATE = 4 * M_HEADS
OFF_MERGE = OFF_GATE + N_GATE
IN_WIDTH = OFF_MERGE + 2 * D_MODEL

N_EXPERTS = 32
TOP_K = 4
D_FF = D_MODEL
SWIGLU_LIMIT = 7.0
SWIGLU_ALPHA = 1.702
MOE_BLOCK = 512

kernel_name = 'hybrid_pool_mlstm_moe_dit'


def rmsnorm(x, w):
    xf = x.astype(jnp.float32)
    y = xf * lax.rsqrt(jnp.mean(xf * xf, axis=-1, keepdims=True) + EPS)
    return (y * w.astype(jnp.float32)).astype(x.dtype)


def modulate(h, shift, scale):
    return h * (1.0 + scale) + shift


def centred_pool(u):
    W = u.shape[2]
    pos = np.arange(W)
    outs = []
    for g, win in enumerate(POOL_WINDOWS):
        ug = u[..., g * POOL_GC:(g + 1) * POOL_GC].astype(jnp.float32)
        cs = jnp.pad(jnp.cumsum(ug, axis=2), ((0, 0), (0, 0), (1, 0), (0, 0)))
        lo = np.clip(pos - win // 2, 0, W)
        hi = np.clip(pos + win // 2, 0, W)
        cnt = (hi - lo).astype(np.float32)[:, None]
        mean = (jnp.take(cs, hi, axis=2) - jnp.take(cs, lo, axis=2)) / cnt
        outs.append(mean - ug)
    return jnp.concatenate(outs, axis=-1).astype(u.dtype)


def short_conv(u, w, b):
    L = u.shape[1]
    pad = CONV_W // 2
    up = jnp.pad(u, ((0, 0), (pad, pad), (0, 0)))
    acc = b
    for j in range(CONV_W):
        acc = acc + up[:, j:j + L] * w[j]
    return jax.nn.silu(acc)


def to_heads(a):
    B, L, _ = a.shape
    return a.reshape(B, L, M_HEADS, M_HEAD_DIM).transpose(0, 2, 1, 3).astype(jnp.float32)


def mlstm_inputs(p, conv_w, conv_b, gate_b):
    qk = short_conv(p[..., OFF_Q:OFF_V], conv_w, conv_b)
    q = to_heads(qk[..., :M_WIDTH]) * (M_HEAD_DIM ** -0.5)
    k = to_heads(qk[..., M_WIDTH:])
    v = to_heads(p[..., OFF_V:OFF_O])
    B, L, _ = p.shape
    g = (p[..., OFF_GATE:OFF_MERGE] + gate_b).astype(jnp.float32)
    g = g.reshape(B, L, 4, M_HEADS).transpose(2, 0, 3, 1)
    return (q, k, v, g[0], jax.nn.log_sigmoid(g[1]), g[2], jax.nn.log_sigmoid(g[3]))


def zero_state(B):
    return (jnp.zeros((B, M_HEADS, M_HEAD_DIM, M_HEAD_DIM), jnp.float32),
            jnp.zeros((B, M_HEADS, M_HEAD_DIM), jnp.float32),
            jnp.zeros((B, M_HEADS), jnp.float32))


def mlstm_scan(q, k, v, ig, lf, state):
    B, H, L, Dh = q.shape
    N = L // CHUNK

    def chunks(a):
        return jnp.moveaxis(a.reshape(a.shape[:2] + (N, CHUNK) + a.shape[3:]), 2, 0)

    mask = jnp.asarray(np.tril(np.ones((CHUNK, CHUNK), dtype=bool)))

    def step(carry, xs):
        C, n, m = carry
        qc, kc, vc, ic, fc = xs
        b = jnp.cumsum(fc, axis=-1)
        logw = jnp.where(mask, b[..., :, None] - b[..., None, :] + ic[..., None, :], -jnp.inf)
        m_inter = b + m[..., None]
        m_t = jnp.maximum(jnp.max(logw, axis=-1), m_inter)
        s = jnp.einsum('bhtd,bhsd->bhts', qc, kc) * jnp.exp(logw - m_t[..., None])
        decay = jnp.exp(m_inter - m_t)
        num = jnp.einsum('bhts,bhsd->bhtd', s, vc) + decay[..., None] * jnp.einsum('bhtk,bhkv->bhtv', qc, C)
        den = jnp.sum(s, axis=-1) + decay * jnp.einsum('bhtk,bhk->bht', qc, n)
        h = num / jnp.maximum(jnp.abs(den), jnp.exp(-m_t))[..., None]
        btot = b[..., -1]
        log_a = btot[..., None] - b + ic
        m_new = jnp.maximum(btot + m, jnp.max(log_a, axis=-1))
        a = jnp.exp(log_a - m_new[..., None])
        cd = jnp.exp(btot + m - m_new)
        C_new = cd[..., None, None] * C + jnp.einsum('bhs,bhsk,bhsv->bhkv', a, kc, vc)
        n_new = cd[..., None] * n + jnp.einsum('bhs,bhsk->bhk', a, kc)
        return (C_new, n_new, m_new), h

    state, h = lax.scan(step, state, (chunks(q), chunks(k), chunks(v), chunks(ig), chunks(lf)))
    return jnp.moveaxis(h, 0, 2).reshape(B, H, L, Dh), state


def mlstm_bidir(q, k, v, i_f, lf_f, i_b, lf_b, init_f, init_b):
    h_f, st_f = mlstm_scan(q, k, v, i_f, lf_f, init_f)
    fl = lambda a: jnp.flip(a, axis=2)
    h_b, st_b = mlstm_scan(fl(q), fl(k), fl(v), fl(i_b), fl(lf_b), init_b)
    return h_f + fl(h_b), st_f, st_b


def mixer_merge(p, pooled, h_m, w_pool, pool_scale, hnorm_w, w_bp, w_bm, w_out):
    B, L, _ = p.shape
    pg = pooled.reshape(B, L, POOL_GROUPS, POOL_GC)
    ya = jnp.einsum('blgc,gcd->blgd', pg, w_pool).reshape(B, L, POOL_WIDTH) * pool_scale
    hn = h_m * lax.rsqrt(jnp.mean(h_m * h_m, axis=-1, keepdims=True) + EPS)
    hn = hn.transpose(0, 2, 1, 3).reshape(B, L, M_WIDTH).astype(p.dtype) * hnorm_w
    yb = hn * jax.nn.sigmoid(p[..., OFF_O:OFF_GATE])
    ga = jax.nn.sigmoid(p[..., OFF_MERGE:OFF_MERGE + D_MODEL])
    gb = jax.nn.sigmoid(p[..., OFF_MERGE + D_MODEL:])
    y = ga * (ya @ w_bp) + gb * (yb @ w_bm)
    return y @ w_out


def moe(h, w_router, b_router, w1, b1, w2, b2):
    shape = h.shape
    x = h.reshape(-1, D_MODEL)
    T = x.shape[0]
    logits = (x @ w_router + b_router).astype(jnp.float32)
    top_v, top_i = lax.top_k(logits, TOP_K)
    top_w = jax.nn.softmax(top_v, axis=-1)
    TK = T * TOP_K
    flat_e = top_i.reshape(-1)
    flat_t = jnp.arange(TK, dtype=jnp.int32) // TOP_K
    flat_w = top_w.reshape(-1)
    order = jnp.argsort(flat_e)
    se = flat_e[order]
    counts = jnp.bincount(flat_e, length=N_EXPERTS)
    starts = jnp.cumsum(counts) - counts
    pcounts = (counts + MOE_BLOCK - 1) // MOE_BLOCK * MOE_BLOCK
    pends = jnp.cumsum(pcounts)
    pstarts = pends - pcounts
    dest = pstarts[se] + jnp.arange(TK, dtype=jnp.int32) - starts[se]
    n_blocks = (TK + MOE_BLOCK - 1) // MOE_BLOCK + N_EXPERTS
    row_tok = jnp.full((n_blocks * MOE_BLOCK,), T, jnp.int32).at[dest].set(flat_t[order])
    row_w = jnp.zeros((n_blocks * MOE_BLOCK,), jnp.float32).at[dest].set(flat_w[order])
    block_e = jnp.minimum(jnp.searchsorted(pends, jnp.arange(n_blocks) * MOE_BLOCK, side='right'),
                          N_EXPERTS - 1).astype(jnp.int32)
    x_pad = jnp.concatenate([x, jnp.zeros((1, D_MODEL), x.dtype)], axis=0)

    def block(y, xs):
        e, rows, wts = xs
        gu = x_pad[rows] @ w1[e] + b1[e]
        gate = jnp.minimum(gu[..., :D_FF], SWIGLU_LIMIT)
        up = jnp.clip(gu[..., D_FF:], -SWIGLU_LIMIT, SWIGLU_LIMIT)
        act = (up + 1.0) * gate * jax.nn.sigmoid(SWIGLU_ALPHA * gate)
        out = act @ w2[e] + b2[e]
        return y.at[rows].add(out * wts[:, None].astype(out.dtype)), None

    y0 = jnp.zeros((T + 1, D_MODEL), x.dtype)
    y, _ = lax.scan(block, y0, (block_e, row_tok.reshape(n_blocks, MOE_BLOCK),
                                row_w.reshape(n_blocks, MOE_BLOCK)))
    return y[:T].reshape(shape)


def setup_inputs(seed: int = 0) -> dict:
    key = jax.random.key(seed)
    ks = jax.random.split(key, 26)
    nrm = lambda k, s, sc: jax.random.normal(k, s, jnp.float32) * sc
    D, L_ = D_MODEL, DEPTH
    gate_base = jnp.concatenate([jnp.zeros((M_HEADS,)), jnp.full((M_HEADS,), F_BIAS_INIT),
                                 jnp.zeros((M_HEADS,)), jnp.full((M_HEADS,), F_BIAS_INIT)]).astype(jnp.float32)
    return {
        'x': nrm(ks[0], (BATCH, SEQ, D), 1.0),
        'c': nrm(ks[1], (BATCH, D), 1.0),
        'ctx': nrm(ks[2], (BATCH, CTX_LEN, D), 1.0),
        'c_ctx': nrm(ks[3], (D,), 1.0),
        'w_ada': nrm(ks[4], (L_, D, 6 * D), 0.5 * D ** -0.5),
        'b_ada': nrm(ks[5], (L_, 6 * D), 0.01),
        'norm1_w': 1.0 + nrm(ks[6], (L_, D), 0.01),
        'w_in': nrm(ks[7], (L_, D, IN_WIDTH), D ** -0.5),
        'gate_b': gate_base + nrm(ks[8], (L_, N_GATE), 0.1),
        'conv_w': nrm(ks[9], (L_, CONV_W, 2 * M_WIDTH), CONV_W ** -0.5),
        'conv_b': nrm(ks[10], (L_, 2 * M_WIDTH), 0.01),
        'w_pool': nrm(ks[11], (L_, POOL_GROUPS, POOL_GC, POOL_GC), POOL_GC ** -0.5),
        'pool_scale': 1.0 + nrm(ks[12], (L_, POOL_WIDTH), 0.1),
        'hnorm_w': 1.0 + nrm(ks[13], (L_, M_WIDTH), 0.01),
        'w_bp': nrm(ks[14], (L_, POOL_WIDTH, D), POOL_WIDTH ** -0.5),
        'w_bm': nrm(ks[15], (L_, M_WIDTH, D), M_WIDTH ** -0.5),
        'w_out': nrm(ks[16], (L_, D, D), D ** -0.5),
        'norm2_w': 1.0 + nrm(ks[17], (L_, D), 0.01),
        'w_router': nrm(ks[18], (L_, D, N_EXPERTS), D ** -0.5),
        'b_router': nrm(ks[19], (L_, N_EXPERTS), 0.01),
        'w1': nrm(ks[20], (L_, N_EXPERTS, D, 2 * D_FF), D ** -0.5),
        'b1': nrm(ks[21], (L_, N_EXPERTS, 2 * D_FF), 0.01),
        'w2': nrm(ks[22], (L_, N_EXPERTS, D_FF, D), D_FF ** -0.5),
        'b2': nrm(ks[23], (L_, N_EXPERTS, D), 0.01),
        'final_norm_w': 1.0 + nrm(ks[24], (D,), 0.01),
    }


def reference(x, c, ctx, c_ctx, w_ada, b_ada, norm1_w, w_in, gate_b, conv_w, conv_b, w_pool,
              pool_scale, hnorm_w, w_bp, w_bm, w_out, norm2_w, w_router, b_router, w1, b1, w2, b2,
              final_norm_w):
    B, L, _ = x.shape
    rows = L // GRID_W
    n_ctx = ctx.shape[1]
    sc = jax.nn.silu(c)
    scx = jax.nn.silu(c_ctx)
    for layer in range(DEPTH):
        mod = (sc @ w_ada[layer] + b_ada[layer])[:, None, :]
        mod_c = scx @ w_ada[layer] + b_ada[layer]
        sh1, s1, g1, sh2, s2, g2 = jnp.split(mod, 6, axis=-1)
        csh1, cs1, cg1, csh2, cs2, cg2 = jnp.split(mod_c, 6, axis=-1)

        p_lat = modulate(rmsnorm(x, norm1_w[layer]), sh1, s1) @ w_in[layer]
        p_ctx = modulate(rmsnorm(ctx, norm1_w[layer]), csh1, cs1) @ w_in[layer]
        ctx_in = mlstm_inputs(p_ctx, conv_w[layer], conv_b[layer], gate_b[layer])
        lat_in = mlstm_inputs(p_lat, conv_w[layer], conv_b[layer], gate_b[layer])
        z = zero_state(B)
        h_ctx, st_f, st_b = mlstm_bidir(*ctx_in, z, z)
        h_lat, _, _ = mlstm_bidir(*lat_in, st_f, st_b)
        pooled = centred_pool(p_lat[..., :POOL_WIDTH].reshape(B, rows, GRID_W, POOL_WIDTH))
        pooled = pooled.reshape(B, L, POOL_WIDTH)
        x = x + g1 * mixer_merge(p_lat, pooled, h_lat, w_pool[layer], pool_scale[layer], hnorm_w[layer],
                                 w_bp[layer], w_bm[layer], w_out[layer])
        if layer < DEPTH - 1:
            pooled_c = centred_pool(p_ctx[:, None, :, :POOL_WIDTH]).reshape(B, n_ctx, POOL_WIDTH)
            ctx = ctx + cg1 * mixer_merge(p_ctx, pooled_c, h_ctx, w_pool[layer], pool_scale[layer],
                                          hnorm_w[layer], w_bp[layer], w_bm[layer], w_out[layer])
            ctx = ctx + cg2 * moe(modulate(rmsnorm(ctx, norm2_w[layer]), csh2, cs2), w_router[layer],
                                  b_router[layer], w1[layer], b1[layer], w2[layer], b2[layer])

        x = x + g2 * moe(modulate(rmsnorm(x, norm2_w[layer]), sh2, s2), w_router[layer], b_router[layer],
                         w1[layer], b1[layer], w2[layer], b2[layer])
    return rmsnorm(x, final_norm_w)
```

```python
from contextlib import ExitStack
import numpy as np
import concourse.bass as bass
import concourse.mybir as mybir
from concourse.bass_utils import run_bass_kernel_spmd

F32 = mybir.dt.float32
BF16 = mybir.dt.bfloat16
AF = mybir.ActivationFunctionType
ALU = mybir.AluOpType

D = 1024
CTX = 256
NE = 32
EPS = 1e-6
INW = 4624
ENGS = ("pe", "act", "dve", "pool", "sp")
SEM_LIMIT = 30000


class Res:
    __slots__ = ("name", "last_w", "readers", "dma_sem", "dma_cnt")

    def __init__(self, name):
        self.name = name
        self.last_w = None
        self.readers = []
        self.dma_sem = None
        self.dma_cnt = 0


class Op:
    __slots__ = ("eng", "fn", "deps", "idx", "signal", "sig", "dma_res", "dma_cnt", "known", "waits")

    def __init__(self, eng, fn):
        self.eng = eng
        self.fn = fn
        self.deps = []
        self.idx = -1
        self.signal = False
        self.sig = 0
        self.dma_res = None
        self.dma_cnt = 0
        self.known = None
        self.waits = []


class Sched:
    def __init__(self, nc):
        self.nc = nc
        self.ops = {e: [] for e in ENGS}
        self.all = []
        self.res = {}
        self.since_bar = []

    def R(self, name):
        r = self.res.get(name)
        if r is None:
            r = Res(name)
            self.res[name] = r
        return r

    def add(self, eng, fn, reads=(), writes=(), dma=None):
        op = Op(eng, fn)
        deps = {}
        for r in reads:
            if r.last_w is not None:
                deps[id(r.last_w)] = r.last_w
        for r in writes:
            if r.last_w is not None:
                deps[id(r.last_w)] = r.last_w
            for q in r.readers:
                deps[id(q)] = q
        for r in reads:
            r.readers.append(op)
        for r in writes:
            r.last_w = op
            r.readers = []
        deps.pop(id(op), None)
        op.deps = list(deps.values())
        if dma is not None:
            op.dma_res = dma
            dma.dma_cnt += 1
            op.dma_cnt = dma.dma_cnt
        op.idx = len(self.ops[eng])
        self.ops[eng].append(op)
        self.all.append(op)
        self.since_bar.append(op)
        return op

    def barrier(self):
        deps = {}
        for op in self.since_bar:
            if op.dma_res is not None:
                k = ("dma", op.dma_res.name)
            else:
                k = op.eng
            deps[k] = op
        dl = list(deps.values())
        self.since_bar = []
        for e in ENGS:
            op = Op(e, None)
            op.deps = list(dl)
            op.idx = len(self.ops[e])
            self.ops[e].append(op)
            self.all.append(op)
            self.since_bar.append(op)
        for r in self.res.values():
            r.last_w = None
            r.readers = []

    def finalize(self):
        known = {e: {} for e in ENGS}
        for op in self.all:
            kn = known[op.eng]
            waits = {}
            for d in op.deps:
                if d.dma_res is not None:
                    key = ("dma", d.dma_res.name)
                    val = d.dma_cnt
                else:
                    if d.eng == "pe" and op.eng == "pe":
                        continue
                    key = d.eng
                    val = d.idx + 1
                if kn.get(key, 0) >= val:
                    continue
                if waits.get(key, (0, None))[0] < val:
                    waits[key] = (val, d)
            op.waits = []
            if waits:
                kn = dict(kn)
                known[op.eng] = kn
            for key, (val, d) in waits.items():
                kn[key] = max(kn.get(key, 0), val)
                if d.known is not None:
                    for k2, v2 in d.known.items():
                        if kn.get(k2, 0) < v2:
                            kn[k2] = v2
                if d.dma_res is None:
                    d.signal = True
                op.waits.append(d)
            op.known = kn
        self.nsig = {}
        for e in ENGS:
            c = 0
            for op in self.ops[e]:
                if op.signal and op.dma_res is None:
                    c += 1
                    op.sig = c
            self.nsig[e] = c

    def emit(self, stack):
        nc = self.nc
        self.finalize()
        sems = {}

        def esem(e, epoch):
            k = (e, epoch)
            if k not in sems:
                sems[k] = stack.enter_context(nc.semaphore(f"s_{e}_{epoch}"))
            return sems[k]

        for e in ENGS:
            for ep in range((self.nsig[e] + SEM_LIMIT - 1) // SEM_LIMIT):
                esem(e, ep)
        for r in self.res.values():
            if r.dma_cnt > 0:
                r.dma_sem = stack.enter_context(nc.semaphore(f"d_{r.name}"))
        block = stack.enter_context(nc.Block())

        def run(eng_name, eng):
            for op in self.ops[eng_name]:
                for d in op.waits:
                    if d.dma_res is not None:
                        eng.wait_ge(d.dma_res.dma_sem, 16 * d.dma_cnt)
                    else:
                        s = d.sig - 1
                        eng.wait_ge(esem(d.eng, s // SEM_LIMIT), (s % SEM_LIMIT) + 1)
                if op.fn is None:
                    if op.signal:
                        s = op.sig - 1
                        eng.sem_inc(esem(op.eng, s // SEM_LIMIT), 1)
                    continue
                ins = op.fn(eng)
                if op.dma_res is not None:
                    ins.then_inc(op.dma_res.dma_sem, 16)
                elif op.signal:
                    s = op.sig - 1
                    ins.then_inc(esem(op.eng, s // SEM_LIMIT), 1)

        @block.tensor
        def _(eng):
            run("pe", eng)

        @block.scalar
        def _(eng):
            run("act", eng)

        @block.vector
        def _(eng):
            run("dve", eng)

        @block.gpsimd
        def _(eng):
            run("pool", eng)

        @block.sync
        def _(eng):
            run("sp", eng)


class Tile:
    __slots__ = ("ap", "r")

    def __init__(self, ap, r):
        self.ap = ap
        self.r = r

    def __getitem__(self, k):
        return self.ap[k]


def build(L, debug=False, ne_run=NE):
    NT = L // 512
    NCH = L // 128
    LT = CTX + L
    NGC = LT // 128
    NG = L // 1024
    nc = bass.Bass("TRN2", target_bir_lowering=False)

    def din(name, shape, dt=F32):
        return nc.dram_tensor(name, list(shape), dt, kind="ExternalInput").ap()

    x = din("x", [L, D]); ctx = din("ctx", [CTX, D]); cT_d = din("cT", [128, 8, 2])
    w_ada = din("w_ada", [D, 6 * D]); b_ada_c = din("b_ada_c", [128, 48])
    n1c_d = din("n1c", [128, 8]); n2c_d = din("n2c", [128, 8]); fnwb_d = din("fnwb", [128, D])
    w_in = din("w_in", [D, INW]); gbc_d = din("gbc", [16, 1]); cwc_d = din("cwc", [128, 8, 3]); cbc_d = din("cbc", [128, 8])
    w_pool = din("w_pool", [4, 128, 128]); psc_d = din("psc", [128, 4]); hnc_d = din("hnc", [128, 4])
    w_bp = din("w_bp", [512, D]); w_bm = din("w_bm", [512, D]); w_out = din("w_out", [D, D])
    w_router = din("w_router", [D, NE]); brb_d = din("brb", [128, NE])
    w1 = din("w1", [NE, D, 2 * D]); b1c_d = din("b1c", [128, NE, 16]); w2 = din("w2", [NE, D, D]); b2_d = din("b2", [NE, D])
    ident_d = din("ident", [128, 128]); maskf_d = din("maskf", [128, 128]); maskb_d = din("maskb", [128, 128])
    pmat_d = din("pmat", [128, 4, 128]); sel_d = din("sel", [16, 16, 128])
    out = nc.dram_tensor("out", [L, D], F32, kind="ExternalOutput").ap()

    def dscr(name, shape, dt):
        return nc.dram_tensor(name, list(shape), dt, kind="ExternalOutput" if debug else "Internal").ap()

    S_qk = dscr("S_qk", [1024, LT], BF16); S_v = dscr("S_v", [LT, 512], BF16); S_g = dscr("S_g", [16, LT], F32)
    S_o = dscr("S_o", [512, L], BF16); S_gb = dscr("S_gb", [1024, L], BF16); S_za = dscr("S_za", [1024, L], BF16)
    S_yb = dscr("S_yb", [512, L], BF16)

    S = Sched(nc)
    R = S.R
    with ExitStack() as st:
        ARW = 53000
        arena = st.enter_context(nc.sbuf_tensor("arena", [128, ARW], F32))
        state = {"off": 0, "gen": 0}

        def T(name, shape, dt=F32, parts=128, res=None):
            n = 1
            for s_ in shape:
                n *= s_
            words = (n + 1) // 2 if dt == BF16 else n
            off = state["off"]
            assert off + words <= ARW, (name, off, words)
            state["off"] = off + words
            ap = arena[0:parts, off:off + words]
            if dt == BF16:
                ap = ap.bitcast(BF16)
                if n % 2:
                    ap = ap[:, 0:n]
            if len(shape) == 2:
                ap = ap.rearrange("p (a b) -> p a b", a=shape[0])
            elif len(shape) == 3:
                ap = ap.rearrange("p (a b c) -> p a b c", a=shape[0], b=shape[1])
            return Tile(ap, R(res or f"{name}.{state['gen']}"))

        banks = [st.enter_context(nc.psum_tensor(f"bank{i}", [128, 512], F32)) for i in range(8)]
        bstate = {"i": 0}

        def bank():
            i = bstate["i"]
            bstate["i"] = (i + 1) % 8
            return Tile(banks[i][:], R(f"bank{i}"))

        def RS(ts):
            return [t.r if isinstance(t, Tile) else t for t in ts]

        def dma(q, o, i, reads=(), writes=(), sem=None):
            S.add(q, lambda e: e.dma_start(out=o, in_=i), RS(reads), RS(writes), dma=sem.r if isinstance(sem, Tile) else sem)

        def mm(o, l, r_, start, stop, reads, writes):
            S.add("pe", lambda e: e.matmul(o, lhsT=l, rhs=r_, start=start, stop=stop), RS(reads), RS(writes))

        def tr(o, i, idn, reads, writes):
            S.add("pe", lambda e: e.transpose(out=o, in_=i, identity=idn), RS(reads), RS(writes))

        def act(o, i, func, reads, writes, bias=None, scale=None, accum=None):
            kw = {}
            if bias is not None:
                kw["bias"] = bias
            if scale is not None:
                kw["scale"] = scale
            if accum is not None:
                kw["accum_out"] = accum
            S.add("act", lambda e: e.activation(out=o, in_=i, func=func, **kw), RS(reads), RS(writes))

        def ts(eng, o, i, s1, s2, op0, op1, reads, writes):
            if op1 is None:
                S.add(eng, lambda e: e.tensor_scalar(out=o, in0=i, scalar1=s1, scalar2=None, op0=op0), RS(reads), RS(writes))
            else:
                S.add(eng, lambda e: e.tensor_scalar(out=o, in0=i, scalar1=s1, scalar2=s2, op0=op0, op1=op1), RS(reads), RS(writes))

        def tt(eng, o, a, b, op, reads, writes):
            S.add(eng, lambda e: e.tensor_tensor(out=o, in0=a, in1=b, op=op), RS(reads), RS(writes))

        def stt(eng, o, a, sc, b, op0, op1, reads, writes):
            S.add(eng, lambda e: e.scalar_tensor_tensor(out=o, in0=a, scalar=sc, in1=b, op0=op0, op1=op1), RS(reads), RS(writes))

        def cp(eng, o, i, reads, writes):
            S.add(eng, lambda e: e.tensor_copy(out=o, in_=i), RS(reads), RS(writes))

        def ms(eng, o, v, writes):
            S.add(eng, lambda e: e.memset(o, v), (), RS(writes))

        def recip(o, i, reads, writes):
            S.add("dve", lambda e: e.reciprocal(out=o, in_=i), RS(reads), RS(writes))

        identf = T("identf", [128]); identb = T("identb", [128], BF16); onesf = T("onesf", [128])
        maskf = T("maskf", [128]); maskb = T("maskb", [128])
        modc = T("modc", [48, 2]); a1 = T("a1", [8]); ca1 = T("ca1", [8]); a2 = T("a2", [8])
        n1c = T("n1c", [8]); n2c = T("n2c", [8]); bac = T("bac", [48])
        gbc = T("gbc", [1], parts=16); cwc = T("cwc", [8, 3]); cbc = T("cbc", [8]); psc = T("psc", [4]); hnc = T("hnc", [4])
        ssq = T("ssq", [8]); std = T("std", [8]); rstd = T("rstd", [8])
        junkb = T("junkb", [D], BF16)
        PERSIST = state["off"]

        for t_, d_ in ((identf, ident_d), (maskf, maskf_d), (maskb, maskb_d), (n1c, n1c_d), (n2c, n2c_d), (bac, b_ada_c),
                       (cwc, cwc_d), (cbc, cbc_d), (psc, psc_d), (hnc, hnc_d)):
            dma("sp", t_.ap, d_, writes=[t_], sem=t_)
        dma("sp", gbc.ap, gbc_d, writes=[gbc], sem=gbc)
        cp("dve", identb.ap, identf.ap, [identf], [identb])
        ms("dve", onesf.ap, 1.0, [onesf])

        def reset_arena():
            S.barrier()
            state["off"] = PERSIST
            state["gen"] += 1

        cT = T("cT", [8, 2]); sg0 = T("sg0", [8, 2]); scT = T("scT", [8, 2], BF16)
        dma("sp", cT.ap, cT_d, writes=[cT], sem=cT)
        act(sg0.ap, cT.ap, AF.Sigmoid, [cT], [sg0])
        tt("dve", scT.ap, cT.ap, sg0.ap, ALU.mult, [cT, sg0], [scT])
        wa = [T(f"wa{i}", [8, 512], BF16) for i in range(2)]
        psA = bank()
        for nt in range(12):
            w_ = wa[nt % 2]
            dma("pool", w_.ap, w_ada[:, nt * 512:(nt + 1) * 512].rearrange("(kc p) n -> p kc n", p=128), writes=[w_], sem=w_)
            for cc in range(4):
                idx = nt * 4 + cc
                for kc in range(8):
                    mm(psA[:, idx * 2:idx * 2 + 2], w_[:, kc, cc * 128:(cc + 1) * 128], scT[:, kc, :], kc == 0, kc == 7, [w_, scT], [psA])
        tt("dve", modc.ap, psA[:, 0:96].rearrange("p (a b) -> p a b", b=2), bac.ap.unsqueeze(2).to_broadcast([128, 48, 2]), ALU.add, [psA, bac], [modc])
        stt("dve", a1.ap, modc[:, 8:16, 0], 1.0, n1c.ap, ALU.add, ALU.mult, [modc, n1c], [a1])
        stt("dve", ca1.ap, modc[:, 8:16, 1], 1.0, n1c.ap, ALU.add, ALU.mult, [modc, n1c], [ca1])
        stt("dve", a2.ap, modc[:, 32:40, 0], 1.0, n2c.ap, ALU.add, ALU.mult, [modc, n2c], [a2])
        reset_arena()
        win = T("win", [8, INW], BF16)
        for kc in range(8):
            dma("pool", win[:, kc, :], w_in[kc * 128:(kc + 1) * 128, :], writes=[win], sem=win)
        wbp = T("wbp", [4, D], BF16)
        dma("pool", wbp.ap, w_bp.rearrange("(g p) n -> p g n", p=128), writes=[wbp], sem=wbp)
        wpl = T("wpl", [4, 128], BF16)
        dma("pool", wpl.ap, w_pool.rearrange("g c d -> c g d"), writes=[wpl], sem=wpl)
        pmt = T("pmt", [4, 128], BF16)
        dma("pool", pmt.ap, pmat_d, writes=[pmt], sem=pmt)
        XT = [T(f"xt{i}", [4, D]) for i in range(2)]
        xn = T("xn", [4, D], BF16); hT = T("hT", [8, 512], BF16)
        sqk = T("sqk", [8, 512], BF16); so = T("so", [4, 512], BF16); sgb = T("sgb", [8, 512], BF16); gaT = T("gaT", [8, 512], BF16)
        sza = T("sza", [8, 512], BF16); sv = T("sv", [4, 512], BF16); ub = T("ub", [4, 512], BF16)
        plT = T("plT", [4, 512], BF16); yaT = T("yaT", [4, 512], BF16); sgt = T("sgt", [512], parts=16)

        def norm_transpose(xt, nsub, acol, shap, dst, n, xn):
            ms("dve", ssq[:, 0:nsub], 0.0, [ssq])
            for s_ in range(nsub):
                act(junkb.ap, xt[:, s_, :], AF.Square, [xt], [junkb, ssq], accum=ssq[:, s_:s_ + 1])
            act(std[:, 0:nsub], ssq[:, 0:nsub], AF.Sqrt, [ssq], [std], scale=1.0 / D, bias=EPS)
            recip(rstd[:, 0:nsub], std[:, 0:nsub], [std], [rstd])
            for s_ in range(nsub):
                act(xn[:, s_, :], xt[:, s_, :], AF.Copy, [xt, rstd], [xn], scale=rstd[:, s_:s_ + 1])
            for j in range(8):
                bk = bank()
                bb = bk.ap.bitcast(BF16)
                for s_ in range(nsub):
                    tr(bb[:, s_ * 128:(s_ + 1) * 128], xn[:, s_, j * 128:(j + 1) * 128], identb.ap, [xn, identb], [bk])
                ts("dve", dst[:, j, 0:n], bb[:, 0:n], acol[:, j:j + 1], shap(j), ALU.mult, ALU.add, [bk, a1, ca1, a2, modc], [dst])

        tiles = [(ctx, 0, 2, 0, True)] + [(x, i * 512, 4, CTX + i * 512, False) for i in range(NT)]
        for ti, (src, t0, nsub, gt0, is_ctx) in enumerate(tiles):
            n = nsub * 128
            xt = XT[ti % 2]
            dma("sp", xt[:, 0:nsub, :], src[t0:t0 + n, :].rearrange("(s p) d -> p s d", p=128), writes=[xt], sem=xt)
            col = 1 if is_ctx else 0
            norm_transpose(xt, nsub, ca1 if is_ctx else a1, lambda j, col=col: modc[:, j, col:col + 1], hT, n, xn)

            def fm(col0, m, evac):
                bk = bank()
                for kc in range(8):
                    mm(bk[0:m, 0:n], win[:, kc, col0:col0 + m], hT[:, kc, 0:n], kc == 0, kc == 7, [win, hT], [bk])
                evac(bk)

            for c8 in range(8):
                if is_ctx and c8 < 4:
                    continue
                fm(512 + c8 * 128, 128, lambda bk, c8=c8: act(sqk[:, c8, 0:n], bk[:, 0:n], AF.Copy, [bk], [sqk]))
            if is_ctx:
                dma("sp", S_qk[512:1024, gt0:gt0 + n].rearrange("(c p) t -> p c t", p=128), sqk[:, 4:8, 0:n], reads=[sqk, R("S_qk")], sem=sqk)
            else:
                dma("sp", S_qk[:, gt0:gt0 + n].rearrange("(c p) t -> p c t", p=128), sqk[:, :, 0:n], reads=[sqk, R("S_qk")], sem=sqk)
            fm(2560, 16, lambda bk: cp("dve", sgt[:, 0:n], bk[0:16, 0:n], [bk], [sgt]))
            dma("sp", S_g[:, gt0:gt0 + n], sgt[:, 0:n], reads=[sgt, R("S_g")], sem=sgt)
            for s_ in range(nsub):
                bk = bank()
                for kc in range(8):
                    mm(bk.ap, hT[:, kc, s_ * 128:(s_ + 1) * 128], win[:, kc, 1536:2048], kc == 0, kc == 7, [hT, win], [bk])
                act(sv[:, s_, :], bk.ap, AF.Copy, [bk], [sv])
                if not is_ctx:
                    bk = bank()
                    for kc in range(8):
                        mm(bk.ap, hT[:, kc, s_ * 128:(s_ + 1) * 128], win[:, kc, 0:512], kc == 0, kc == 7, [hT, win], [bk])
                    act(ub[:, s_, :], bk.ap, AF.Copy, [bk], [ub])
            dma("sp", S_v[gt0:gt0 + n, :].rearrange("(s p) d -> p s d", p=128), sv[:, 0:nsub, :], reads=[sv, R("S_v")], sem=sv)
            if is_ctx:
                continue
            for c4 in range(4):
                fm(2048 + c4 * 128, 128, lambda bk, c4=c4: act(so[:, c4, :], bk.ap, AF.Sigmoid, [bk], [so]))
            dma("sp", S_o[:, t0:t0 + n].rearrange("(c p) t -> p c t", p=128), so.ap, reads=[so, R("S_o")], sem=so)
            for c8 in range(8):
                fm(2576 + c8 * 128, 128, lambda bk, c8=c8: act(gaT[:, c8, :], bk.ap, AF.Sigmoid, [bk], [gaT]))
            for c8 in range(8):
                fm(3600 + c8 * 128, 128, lambda bk, c8=c8: act(sgb[:, c8, :], bk.ap, AF.Sigmoid, [bk], [sgb]))
            dma("sp", S_gb[:, t0:t0 + n].rearrange("(c p) t -> p c t", p=128), sgb.ap, reads=[sgb, R("S_gb")], sem=sgb)
            for s_ in range(4):
                bk = bank()
                for g in range(4):
                    mm(bk[:, g * 128:(g + 1) * 128], ub[:, s_, g * 128:(g + 1) * 128], pmt[:, g, :], True, True, [ub, pmt], [bk])
                cp("dve", plT[:, :, s_ * 128:(s_ + 1) * 128], bk.ap.rearrange("p (g t) -> p g t", g=4), [bk], [plT])
            for g in range(4):
                bk = bank()
                mm(bk.ap, wpl[:, g, :], plT[:, g, :], True, True, [wpl, plT], [bk])
                act(yaT[:, g, :], bk.ap, AF.Copy, [bk, psc], [yaT], scale=psc[:, g:g + 1])
            for dc in range(8):
                bk = bank()
                for g in range(4):
                    mm(bk.ap, wbp[:, g, dc * 128:(dc + 1) * 128], yaT[:, g, :], g == 0, g == 3, [wbp, yaT], [bk])
                tt("dve", sza[:, dc, :], bk.ap, gaT[:, dc, :], ALU.mult, [bk, gaT], [sza])
            dma("sp", S_za[:, t0:t0 + n].rearrange("(c p) t -> p c t", p=128), sza.ap, reads=[sza, R("S_za")], sem=sza)

        reset_arena()
        sel = T("sel", [16, 128], parts=16); nsel = T("nsel", [16, 128], parts=16)
        dma("sp", sel.ap, sel_d, writes=[sel], sem=sel)
        ts("dve", nsel.ap, sel.ap, -1.0, None, ALU.mult, None, [sel], [nsel])
        qp = {d_: T(f"qp{d_}", [LT], BF16) for d_ in "fb"}
        kp = {d_: T(f"kp{d_}", [LT], BF16) for d_ in "fb"}
        k2 = {d_: T(f"k2{d_}", [NGC, 128], BF16) for d_ in "fb"}
        vext = T("vext", [NGC, 129], BF16)
        hacc = T("hacc", [NCH, 128])
        EB = {d_: T(f"EB{d_}", [NGC]) for d_ in "fb"}
        raw = {w_: T(f"raw{w_}", [514], BF16) for w_ in "qk"}
        accb = T("accb", [512]); qc = T("qc", [512]); kc_ = T("kc", [512])
        Gt = T("Gt", [512], parts=16); Pt = T("Pt", [512], parts=16)
        F0 = T("F0", [512], parts=16); F1 = T("F1", [512], parts=16); B0 = T("B0", [512], parts=16); B1 = T("B1", [512], parts=16)
        Fe = T("Fe", [512], parts=16); Be = T("Be", [512], parts=16)
        EE = [T(f"EE{i}", [512]) for i in range(2)]; k2T = T("k2T", [512], BF16)
        erot = [0]

        def nextE():
            erot[0] += 1
            return EE[erot[0] % 2]
        Cst = {d_: T(f"C{d_}", [129]) for d_ in "fb"}
        Cbf = {d_: T(f"Cbf{d_}", [129], BF16) for d_ in "fb"}
        SM = [T(f"SM{i}", [128], BF16) for i in range(4)]
        dab = [T(f"dab{i}", [1]) for i in range(4)]
        rdn = [T(f"rdn{i}", [1]) for i in range(4)]
        ssh = T("ssh", [NCH]); sth = T("sth", [NCH]); rsh = T("rsh", [NCH])
        hn = [T(f"hn{i}", [4, 128], BF16) for i in range(1)]
        sgo = [T(f"sgo{i}", [512], BF16) for i in range(1)]
        ybt = [T(f"ybt{i}", [512], BF16) for i in range(1)]
        mask = {"f": maskf, "b": maskb}

        blocks = [(0, CTX, True)] + [(CTX + 512 * i, 512, False) for i in range(NT)]
        for h in range(4):
            dma("sp", vext[:, :, 0:128], S_v[:, h * 128:(h + 1) * 128].rearrange("(c p) d -> p c d", p=128), reads=[R("S_v")], writes=[vext], sem=vext)
            ms("dve", vext[:, :, 128:129], 1.0, [vext])
            def b1_block(bix):
                a, n, is_ctx = blocks[bix]
                nch = n // 128
                gc0 = a // 128
                s0 = a in (0, CTX)
                s1 = (a + n) in (CTX, LT)
                for wname in "qk":
                    if is_ctx and wname == "q":
                        continue
                    rw = raw[wname]
                    ci = h if wname == "q" else 4 + h
                    row0 = ci * 128
                    if s0:
                        ms("dve", rw[:, 0:1], 0.0, [rw])
                    if s1:
                        ms("dve", rw[:, n + 1:n + 2], 0.0, [rw])
                    lo = a if s0 else a - 1
                    hi = a + n if s1 else a + n + 1
                    dma("sp", rw[:, lo - (a - 1):hi - (a - 1)], S_qk[row0:row0 + 128, lo:hi], reads=[R("S_qk")], writes=[rw], sem=rw)
                    ts("dve", accb[:, 0:n], rw[:, 1:n + 1], cwc[:, ci, 1:2], cbc[:, ci:ci + 1], ALU.mult, ALU.add, [rw, cwc, cbc], [accb])
                    stt("dve", accb[:, 0:n], rw[:, 0:n], cwc[:, ci, 0:1], accb[:, 0:n], ALU.mult, ALU.add, [rw, cwc, accb], [accb])
                    stt("dve", accb[:, 0:n], rw[:, 2:n + 2], cwc[:, ci, 2:3], accb[:, 0:n], ALU.mult, ALU.add, [rw, cwc, accb], [accb])
                    dst = qc if wname == "q" else kc_
                    act(dst[:, 0:n], accb[:, 0:n], AF.Silu, [accb], [dst])
                dma("sp", Gt[:, 0:n], S_g[:, a:a + n], reads=[R("S_g")], writes=[Gt], sem=Gt)
                ts("dve", Gt[:, 0:n], Gt[:, 0:n], gbc[:, 0:1], None, ALU.add, None, [Gt, gbc], [Gt])
                act(Pt[:, 0:n], Gt[:, 0:n], AF.Exp, [Gt], [Pt], scale=-1.0)
                act(Pt[:, 0:n], Pt[:, 0:n], AF.Ln, [Pt], [Pt], bias=1.0)

                def v3(t_):
                    return t_[:, 0:n].rearrange("p (c t) -> p c t", t=128)

                cur = Pt
                for si, k in enumerate((1, 2, 4, 8, 16, 32, 64)):
                    nx = F0 if si % 2 == 0 else F1
                    tt("dve", v3(nx)[:, :, k:], v3(cur)[:, :, k:], v3(cur)[:, :, :128 - k], ALU.add, [cur], [nx])
                    cp("dve", v3(nx)[:, :, :k], v3(cur)[:, :, :k], [cur], [nx])
                    cur = nx
                CSf = cur
                cur = Pt
                for si, k in enumerate((1, 2, 4, 8, 16, 32, 64)):
                    nx = B0 if si % 2 == 0 else B1
                    tt("pool", v3(nx)[:, :, :128 - k], v3(cur)[:, :, :128 - k], v3(cur)[:, :, k:], ALU.add, [cur], [nx])
                    cp("pool", v3(nx)[:, :, 128 - k:], v3(cur)[:, :, 128 - k:], [cur], [nx])
                    cur = nx
                CSb = cur
                cp("dve", v3(Fe), v3(CSf)[:, :, 127:128].to_broadcast([16, nch, 128]), [CSf], [Fe])
                cp("pool", v3(Be), v3(CSb)[:, :, 0:1].to_broadcast([16, nch, 128]), [CSb], [Be])
                for d_ in "fb":
                    CS, CSe = (CSf, Fe) if d_ == "f" else (CSb, Be)
                    ir = (0 if d_ == "f" else 8) + h
                    fr = (4 if d_ == "f" else 12) + h
                    if not is_ctx:
                        bk = bank()
                        mm(bk[:, 0:n], nsel[:, fr, :], CS[:, 0:n], True, True, [nsel, CS], [bk])
                        EQ = nextE()
                        act(EQ[:, 0:n], bk[:, 0:n], AF.Exp, [bk], [EQ])
                        stt("dve", qp[d_][:, a:a + n], qc[:, 0:n], 128.0 ** -0.5, EQ[:, 0:n], ALU.mult, ALU.mult, [qc, EQ], [R(f'qp{d_}{bix}')])
                        bk = bank()
                        mm(bk[:, 0:n], sel[:, ir, :], Gt[:, 0:n], True, False, [sel, Gt], [bk])
                        mm(bk[:, 0:n], sel[:, fr, :], CS[:, 0:n], False, True, [sel, CS], [bk])
                        EK = nextE()
                        act(EK[:, 0:n], bk[:, 0:n], AF.Exp, [bk], [EK])
                        tt("dve", kp[d_][:, a:a + n], kc_[:, 0:n], EK[:, 0:n], ALU.mult, [kc_, EK], [R(f'kp{d_}{bix}')])
                    bk = bank()
                    mm(bk[:, 0:n], sel[:, ir, :], Gt[:, 0:n], True, False, [sel, Gt], [bk])
                    mm(bk[:, 0:n], sel[:, fr, :], CS[:, 0:n], False, False, [sel, CS], [bk])
                    mm(bk[:, 0:n], nsel[:, fr, :], CSe[:, 0:n], False, True, [nsel, CSe], [bk])
                    EK2 = nextE()
                    act(EK2[:, 0:n], bk[:, 0:n], AF.Exp, [bk], [EK2])
                    tt("dve", k2T[:, 0:n], kc_[:, 0:n], EK2[:, 0:n], ALU.mult, [kc_, EK2], [k2T])
                    bk = bank()
                    bb = bk.ap.bitcast(BF16)
                    for c in range(nch):
                        tr(bb[:, c * 128:(c + 1) * 128], k2T[:, c * 128:(c + 1) * 128], identb.ap, [k2T, identb], [bk])
                    act(k2[d_][:, gc0:gc0 + nch, :], bb[:, 0:n].rearrange("p (c t) -> p c t", t=128), AF.Copy, [bk], [R(f'k2{d_}{bix}')])
                    bk = bank()
                    ecol = v3(CS)[:, :, 127] if d_ == "f" else v3(CS)[:, :, 0]
                    mm(bk[:, 0:nch], nsel[:, fr, :], ecol, True, True, [nsel, CS], [bk])
                    act(EB[d_][:, gc0:gc0 + nch], bk[:, 0:nch], AF.Exp, [bk], [R(f'EB{d_}{bix}')])

            for d_ in "fb":
                ms("dve", Cst[d_].ap, 0.0, [Cst[d_]])

            def blk_of(gc):
                return 0 if gc < 2 else 1 + (gc - 2) // 4

            def state_update(d_, gc):
                bx = blk_of(gc)
                bk = bank()
                mm(bk[:, 0:129], k2[d_][:, gc, :], vext[:, gc, :], True, True, [R(f'k2{d_}{bx}'), vext], [bk])
                stt("dve", Cst[d_].ap, Cst[d_].ap, EB[d_][:, gc:gc + 1], bk[:, 0:129], ALU.mult, ALU.add, [Cst[d_], R(f'EB{d_}{bx}'), bk], [Cst[d_]])


            def chunk_step(d_, c):
                gc = 2 + c
                tk = slice(gc * 128, (gc + 1) * 128)
                i_ = rot[0] % 4
                rot[0] += 1
                b1_ = bank()
                bx = blk_of(gc)
                mm(b1_[:, 0:128], kp[d_][:, tk], qp[d_][:, tk], True, True, [R(f'kp{d_}{bx}'), R(f'qp{d_}{bx}')], [b1_])
                tt("dve", SM[i_].ap, b1_[:, 0:128], mask[d_].ap, ALU.mult, [b1_, mask[d_]], [SM[i_]])
                b2_ = bank()
                mm(b2_[:, 0:129], SM[i_].ap, vext[:, gc, :], True, False, [SM[i_], vext], [b2_])
                mm(b2_[:, 0:129], qp[d_][:, tk], Cbf[d_].ap, False, True, [R(f'qp{d_}{bx}'), Cbf[d_]], [b2_])
                act(dab[i_].ap, b2_[:, 128:129], AF.Abs, [b2_], [dab[i_]])
                ts("dve", dab[i_].ap, dab[i_].ap, 1.0, None, ALU.max, None, [dab[i_]], [dab[i_]])
                recip(rdn[i_].ap, dab[i_].ap, [dab[i_]], [rdn[i_]])
                hr = R(f"hacc{c}")
                if c not in seen:
                    seen.add(c)
                    act(hacc[:, c, :], b2_[:, 0:128], AF.Copy, [b2_, rdn[i_]], [hr], scale=rdn[i_][:, 0:1])
                else:
                    stt("dve", hacc[:, c, :], b2_[:, 0:128], rdn[i_][:, 0:1], hacc[:, c, :], ALU.mult, ALU.add, [b2_, rdn[i_], hr], [hr])
                state_update(d_, gc)
                act(Cbf[d_].ap, Cst[d_].ap, AF.Copy, [Cst[d_]], [Cbf[d_]])

            def b3_block(bi):
                hn_ = hn[0]; sg_ = sgo[0]; yb_ = ybt[0]
                c0 = bi * 4
                hr4 = [R(f"hacc{c}") for c in range(c0, c0 + 4)]
                ms("dve", ssh[:, c0:c0 + 4], 0.0, [ssh])
                for c in range(c0, c0 + 4):
                    act(junkb[:, 0:128], hacc[:, c, :], AF.Square, [R(f"hacc{c}")], [junkb, ssh], accum=ssh[:, c:c + 1])
                act(sth[:, c0:c0 + 4], ssh[:, c0:c0 + 4], AF.Sqrt, [ssh], [sth], scale=1.0 / 128, bias=EPS)
                recip(rsh[:, c0:c0 + 4], sth[:, c0:c0 + 4], [sth], [rsh])
                dma("sp", sg_.ap, S_o[h * 128:(h + 1) * 128, bi * 512:(bi + 1) * 512], reads=[R("S_o")], writes=[sg_], sem=sg_)
                tt("dve", hn_.ap, hacc[:, c0:c0 + 4, :], rsh[:, c0:c0 + 4].unsqueeze(2).to_broadcast([128, 4, 128]), ALU.mult, hr4 + [rsh.r], [hn_])
                bk = bank()
                bb = bk.ap.bitcast(BF16)
                for c in range(4):
                    tr(bb[:, c * 128:(c + 1) * 128], hn_[:, c, :], identb.ap, [hn_, identb], [bk])
                stt("dve", yb_.ap, bb[:, 0:512], hnc[:, h:h + 1], sg_.ap, ALU.mult, ALU.mult, [bk, hnc, sg_], [yb_])
                dma("sp", S_yb[h * 128:(h + 1) * 128, bi * 512:(bi + 1) * 512], yb_.ap, reads=[yb_, R("S_yb")], sem=yb_)

            b1_block(0)
            done_b1 = set()
            for bx_ in (1, NT):
                if bx_ not in done_b1:
                    b1_block(bx_); done_b1.add(bx_)
            for gc in (0, 1):
                state_update("f", gc)
            for gc in (1, 0):
                state_update("b", gc)
            for d_ in "fb":
                act(Cbf[d_].ap, Cst[d_].ap, AF.Copy, [Cst[d_]], [Cbf[d_]])
            seen = set()
            rot = [0]
            for i in range(NT):
                for cc_ in range(4):
                    chunk_step("f", i * 4 + cc_)
                    chunk_step("b", (NT - 1 - i) * 4 + 3 - cc_)
                for bx_ in (i + 2, NT - 1 - i):
                    if 1 <= bx_ <= NT and bx_ not in done_b1:
                        b1_block(bx_); done_b1.add(bx_)
                if 2 * i + 1 >= NT:
                    for bq in sorted({i, NT - 1 - i}):
                        b3_block(bq)
            assert len(done_b1) == NT

        reset_arena()
        g1b = T("g1b", [D]); g2b = T("g2b", [D]); fnwb = T("fnwb", [D]); brb = T("brb", [NE])
        dma("sp", fnwb.ap, fnwb_d, writes=[fnwb], sem=fnwb)
        dma("sp", brb.ap, brb_d, writes=[brb], sem=brb)
        wbm = T("wbm", [4, D], BF16); wout = T("wout", [8, D], BF16); wrt = T("wrt", [8, NE], BF16)
        dma("pool", wbm.ap, w_bm.rearrange("(g p) n -> p g n", p=128), writes=[wbm], sem=wbm)
        dma("pool", wout.ap, w_out.rearrange("(g p) n -> p g n", p=128), writes=[wout], sem=wout)
        dma("pool", wrt.ap, w_router.rearrange("(g p) n -> p g n", p=128), writes=[wrt], sem=wrt)
        b1c = T("b1c", [NE, 16]); b1p = T("b1p", [NE, 16])
        dma("sp", b1c.ap, b1c_d, writes=[b1c], sem=b1c)
        ts("dve", b1p.ap, b1c.ap, 1.0, None, ALU.add, None, [b1c], [b1p])
        b2g = T("b2g", [D], BF16, parts=32)
        x1 = T("x1", [8, D]); h2T = T("h2T", [8, 1024], BF16)
        wts = T("wts", [8, NE]); wtT = T("wtT", [8, 128], BF16, parts=32)
        lg = T("lg", [NE]); t8 = T("t8", [8]); nm1 = T("nm1", [1]); mk = T("mk", [NE]); ex = T("ex", [NE]); wsum = T("wsum", [1]); rws = T("rws", [1])
        ring = [T(f"ring{i}", [8, D], BF16) for i in range(4)]
        ring_i = [0]

        def next_unit():
            u = ring[ring_i[0] % 4]
            ring_i[0] += 1
            return u
        c_off = state["off"]
        b2f = T("b2f", [D], parts=32)
        dma("sp", b2f.ap, b2_d, writes=[b2f], sem=b2f)
        dg = [T(f"dg{i}", [128]) for i in range(2)]
        for gi, (gt, base) in enumerate(((g1b, 16), (g2b, 40))):
            for hf in range(2):
                bk = bank()
                for jj in range(4):
                    j = hf * 4 + jj
                    d_ = dg[j % 2]
                    ts("dve", d_.ap, identf.ap, modc[:, base + j, 0:1], None, ALU.mult, None, [identf, modc], [d_])
                    mm(bk[:, jj * 128:(jj + 1) * 128], onesf.ap, d_.ap, True, True, [onesf, d_], [bk])
                cp("dve", gt[:, hf * 512:(hf + 1) * 512], bk.ap, [bk], [gt])

        tt("dve", b2g.ap, b2f.ap, g2b[0:32, :], ALU.mult, [b2f, g2b], [b2g])
        state["off"] = c_off
        ybT = T("ybT", [4, 512], BF16, res="cA"); gbT = T("gbT", [8, 512], BF16, res="cB"); zaT = T("zaT", [8, 512], BF16, res="cC")
        yT = T("yT", [8, 512], BF16, res="cD"); tmpA = T("tmpA", [512], res="cE"); tmpB = T("tmpB", [512], res="cF"); xn2 = T("xn2", [4, D], BF16)
        c_end = state["off"]
        state["off"] = c_off
        actT = [T(f"actT{i}", [8, 512], BF16, res=r_) for i, r_ in enumerate(("cA", "cB"))]
        gcb = [T(f"gcb{i}", [512], res=r_) for i, r_ in enumerate(("cC", "cC"))]
        sgb_ = [T(f"sgx{i}", [512], res=r_) for i, r_ in enumerate(("cD", "cD"))]
        ucb = [T(f"ucb{i}", [512], res=r_) for i, r_ in enumerate(("cE", "cE"))]
        t1b = [T(f"t1b{i}", [512], res=r_) for i, r_ in enumerate(("cF", "cF"))]
        state["off"] = max(state["off"], c_end)
        for lst, nm in ((actT, "actT"), (gcb, "gcb"), (sgb_, "sgx"), (ucb, "ucb"), (t1b, "t1b")):
            for i, t_ in enumerate(lst):
                t_.r = R(f"{nm}{i}")
        for t_, nm in ((ybT, "ybT"), (gbT, "gbT"), (zaT, "zaT"), (yT, "yT"), (tmpA, "tmpA"), (tmpB, "tmpB")):
            t_.r = R(nm)

        def load_w1(e, half):
            u = next_unit()
            dma("pool", u.ap, w1[e, :, half * D:(half + 1) * D].rearrange("(kc p) n -> p kc n", p=128), writes=[u], sem=u)
            return u

        def load_w2(e):
            u = next_unit()
            dma("pool", u.ap, w2[e].rearrange("(kc p) n -> p kc n", p=128), writes=[u], sem=u)
            tt("pool", u.ap, u.ap, g2b.ap.unsqueeze(1).to_broadcast([128, 8, D]), ALU.mult, [u, g2b], [u])
            return u

        for g in range(NG):
            S.barrier()
            T0 = g * 1024
            for tti in range(2):
                t0 = T0 + tti * 512
                dma("sp", x1[:, tti * 4:(tti + 1) * 4, :], x[t0:t0 + 512, :].rearrange("(s p) d -> p s d", p=128), writes=[x1], sem=x1)
                dma("sp", ybT.ap, S_yb[:, t0:t0 + 512].rearrange("(c p) t -> p c t", p=128), reads=[R("S_yb")], writes=[ybT], sem=ybT)
                dma("sp", gbT.ap, S_gb[:, t0:t0 + 512].rearrange("(c p) t -> p c t", p=128), reads=[R("S_gb")], writes=[gbT], sem=gbT)
                dma("sp", zaT.ap, S_za[:, t0:t0 + 512].rearrange("(c p) t -> p c t", p=128), reads=[R("S_za")], writes=[zaT], sem=zaT)
                for dc in range(8):
                    bk = bank()
                    for kc in range(4):
                        mm(bk.ap, wbm[:, kc, dc * 128:(dc + 1) * 128], ybT[:, kc, :], kc == 0, kc == 3, [wbm, ybT], [bk])
                    tt("dve", tmpA.ap, bk.ap, gbT[:, dc, :], ALU.mult, [bk, gbT], [tmpA])
                    tt("dve", yT[:, dc, :], tmpA.ap, zaT[:, dc, :], ALU.add, [tmpA, zaT], [yT])
                for s_ in range(4):
                    for hf in range(2):
                        bk = bank()
                        for kc in range(8):
                            mm(bk.ap, yT[:, kc, s_ * 128:(s_ + 1) * 128], wout[:, kc, hf * 512:(hf + 1) * 512], kc == 0, kc == 7, [yT, wout], [bk])
                        xs = x1[:, tti * 4 + s_, hf * 512:(hf + 1) * 512]
                        tt("dve", tmpB.ap, bk.ap, g1b[:, hf * 512:(hf + 1) * 512], ALU.mult, [bk, g1b], [tmpB])
                        tt("dve", xs, xs, tmpB.ap, ALU.add, [x1, tmpB], [x1])
                xv = Tile(x1[:, tti * 4:(tti + 1) * 4, :], x1.r)
                dst = Tile(h2T[:, :, tti * 512:(tti + 1) * 512], h2T.r)
                norm_transpose(xv, 4, a2, lambda j: modc[:, 24 + j, 0:1], dst, 512, xn2)
                for s_ in range(4):
                    sg_ = tti * 4 + s_
                    bk = bank()
                    for kc in range(8):
                        mm(bk[:, 0:NE], h2T[:, kc, sg_ * 128:(sg_ + 1) * 128], wrt[:, kc, :], kc == 0, kc == 7, [h2T, wrt], [bk])
                    tt("dve", lg.ap, bk[:, 0:NE], brb.ap, ALU.add, [bk, brb], [lg])
                    S.add("dve", lambda e: e.max(out=t8.ap, in_=lg.ap), RS([lg]), RS([t8]))
                    ts("dve", nm1.ap, t8[:, 0:1], -1.0, None, ALU.mult, None, [t8], [nm1])
                    ts("dve", mk.ap, lg.ap, t8[:, 3:4], 1e30, ALU.subtract, ALU.mult, [lg, t8], [mk])
                    ts("dve", mk.ap, mk.ap, 1.0, 0.0, ALU.add, ALU.max, [mk], [mk])
                    ts("dve", mk.ap, mk.ap, 1.0, None, ALU.min, None, [mk], [mk])
                    act(ex.ap, lg.ap, AF.Exp, [lg, nm1], [ex], bias=nm1[:, 0:1])
                    tt("dve", ex.ap, ex.ap, mk.ap, ALU.mult, [ex, mk], [ex])
                    S.add("dve", lambda e: e.reduce_sum(out=wsum.ap, in_=ex.ap, axis=mybir.AxisListType.X), RS([ex]), RS([wsum]))
                    recip(rws.ap, wsum.ap, [wsum], [rws])
                    ts("dve", wts[:, sg_, :], ex.ap, rws[:, 0:1], None, ALU.mult, None, [ex, rws], [wts])
                    bk = bank()
                    S.add("pe", lambda e, bk=bk, sg_=sg_: e.transpose(out=bk[0:NE, 0:128], in_=wts[:, sg_, :], identity=identf.ap), RS([wts, identf]), RS([bk]))
                    cp("dve", wtT[:, sg_, :], bk[0:NE, 0:128], [bk], [wtT])
                    for hf in range(2):
                        bk = bank()
                        mm(bk.ap, wtT[:, sg_, :], b2g[:, hf * 512:(hf + 1) * 512], True, True, [wtT, b2g], [bk])
                        xs = x1[:, sg_, hf * 512:(hf + 1) * 512]
                        tt("dve", xs, xs, bk.ap, ALU.add, [x1, bk], [x1])
            S.barrier()
            nxt = [load_w1(0, 0), load_w1(0, 1), load_w2(0)] if ne_run > 0 else None
            for e in range(ne_run):
                ua, ub_, uc = nxt
                if e + 1 < ne_run:
                    nxt = [load_w1(e + 1, 0)]
                for tti in range(2):
                    aT = actT[tti]
                    for fp in range(8):
                        i_ = fp % 2
                        bg = bank()
                        for kc in range(8):
                            mm(bg.ap, ua[:, kc, fp * 128:(fp + 1) * 128], h2T[:, kc, tti * 512:(tti + 1) * 512], kc == 0, kc == 7, [ua, h2T], [bg])
                        bu = bank()
                        for kc in range(8):
                            mm(bu.ap, ub_[:, kc, fp * 128:(fp + 1) * 128], h2T[:, kc, tti * 512:(tti + 1) * 512], kc == 0, kc == 7, [ub_, h2T], [bu])
                        ts("dve", gcb[i_].ap, bg.ap, b1c[:, e, fp:fp + 1], 7.0, ALU.add, ALU.min, [bg, b1c], [gcb[i_]])
                        act(sgb_[i_].ap, gcb[i_].ap, AF.Sigmoid, [gcb[i_]], [sgb_[i_]], scale=1.702)
                        ts("dve", ucb[i_].ap, bu.ap, b1p[:, e, 8 + fp:9 + fp], 8.0, ALU.add, ALU.min, [bu, b1p], [ucb[i_]])
                        tt("dve", t1b[i_].ap, gcb[i_].ap, sgb_[i_].ap, ALU.mult, [gcb[i_], sgb_[i_]], [t1b[i_]])
                        stt("dve", aT[:, fp, :], ucb[i_].ap, -6.0, t1b[i_].ap, ALU.max, ALU.mult, [ucb[i_], t1b[i_]], [aT])
                if e + 1 < ne_run:
                    nxt.append(load_w1(e + 1, 1))
                    nxt.append(load_w2(e + 1))
                for tti in range(2):
                    aT = actT[tti]
                    for s_ in range(4):
                        sg_ = tti * 4 + s_
                        for hf in range(2):
                            bk = bank()
                            for fc in range(8):
                                mm(bk.ap, aT[:, fc, s_ * 128:(s_ + 1) * 128], uc[:, fc, hf * 512:(hf + 1) * 512], fc == 0, fc == 7, [aT, uc], [bk])
                            xs = x1[:, sg_, hf * 512:(hf + 1) * 512]
                            stt("dve", xs, bk.ap, wts[:, sg_, e:e + 1], xs, ALU.mult, ALU.add, [bk, wts, x1], [x1])
            ms("dve", ssq.ap, 0.0, [ssq])
            for s_ in range(8):
                act(junkb.ap, x1[:, s_, :], AF.Square, [x1], [junkb, ssq], accum=ssq[:, s_:s_ + 1])
            act(std.ap, ssq.ap, AF.Sqrt, [ssq], [std], scale=1.0 / D, bias=EPS)
            recip(rstd.ap, std.ap, [std], [rstd])
            for s_ in range(8):
                stt("dve", x1[:, s_, :], x1[:, s_, :], rstd[:, s_:s_ + 1], fnwb.ap, ALU.mult, ALU.mult, [x1, rstd, fnwb], [x1])
            dma("sp", out[T0:T0 + 1024, :].rearrange("(s p) d -> p s d", p=128), x1.ap, reads=[x1, R("out")], sem=x1)
        S.add("sp", None, writes=[R("out")])
        S.emit(st)
    return nc


def _consts():
    ident = np.eye(128, dtype=np.float32)
    s_ = np.arange(128)
    maskf = (s_[:, None] <= s_[None, :]).astype(np.float32)
    maskb = (s_[:, None] >= s_[None, :]).astype(np.float32)
    pm = np.zeros((128, 4, 128), np.float32)
    pos = np.arange(64)
    for g, win in enumerate((2, 4, 8, 16)):
        lo = np.clip(pos - win // 2, 0, 64)
        hi = np.clip(pos + win // 2, 0, 64)
        P = np.zeros((64, 64), np.float32)
        for p in range(64):
            P[lo[p]:hi[p], p] = 1.0 / float(hi[p] - lo[p])
            P[p, p] -= 1.0
        pm[0:64, g, 0:64] = P
        pm[64:128, g, 64:128] = P
    sel = np.zeros((16, 16, 128), np.float32)
    for r in range(16):
        sel[r, r, :] = 1.0
    return ident, maskf, maskb, pm, sel


def _col(v, nchunk):
    return np.ascontiguousarray(np.asarray(v, np.float32).reshape(nchunk, 128).T)


def make_in_maps(inp, L, nb):
    f = lambda a: np.ascontiguousarray(np.asarray(a, dtype=np.float32))
    ident, maskf, maskb, pm, sel = _consts()
    shared = {
        "w_ada": f(inp["w_ada"][0]), "b_ada_c": _col(inp["b_ada"][0], 48),
        "n1c": _col(inp["norm1_w"][0], 8), "n2c": _col(inp["norm2_w"][0], 8),
        "fnwb": f(np.broadcast_to(np.asarray(inp["final_norm_w"], np.float32)[None, :], (128, D))),
        "w_in": f(inp["w_in"][0]), "gbc": f(np.asarray(inp["gate_b"][0]).reshape(16, 1)),
        "cwc": f(np.asarray(inp["conv_w"][0], np.float32).reshape(3, 8, 128).transpose(2, 1, 0)),
        "cbc": _col(inp["conv_b"][0], 8),
        "w_pool": f(inp["w_pool"][0]), "psc": _col(inp["pool_scale"][0], 4), "hnc": _col(inp["hnorm_w"][0], 4),
        "w_bp": f(inp["w_bp"][0]), "w_bm": f(inp["w_bm"][0]), "w_out": f(inp["w_out"][0]),
        "w_router": f(inp["w_router"][0]),
        "brb": f(np.broadcast_to(np.asarray(inp["b_router"][0], np.float32)[None, :], (128, NE))),
        "w1": f(inp["w1"][0]), "b1c": f(np.asarray(inp["b1"][0], np.float32).reshape(NE, 16, 128).transpose(2, 0, 1)),
        "w2": f(inp["w2"][0]), "b2": f(inp["b2"][0]),
        "ident": ident, "maskf": maskf, "maskb": maskb, "pmat": pm, "sel": sel,
    }
    maps = []
    cc = np.asarray(inp["c_ctx"], np.float32)
    for b in range(nb):
        m = dict(shared)
        m["x"] = f(inp["x"][b])
        m["ctx"] = f(inp["ctx"][b])
        cb = np.asarray(inp["c"][b], np.float32)
        m["cT"] = np.ascontiguousarray(np.stack([cb.reshape(8, 128).T, cc.reshape(8, 128).T], axis=-1))
        maps.append(m)
    return maps


def kernel(**inputs):
    xs = np.asarray(inputs["x"])
    B, L, _ = xs.shape
    nc = build(L)
    maps = make_in_maps(inputs, L, B)
    res = run_bass_kernel_spmd(nc, maps, core_ids=list(range(B)))
    return np.stack([np.asarray(r["out"], dtype=np.float32) for r in res.results], axis=0)
```

```python
from contextlib import ExitStack
import numpy as np
import concourse.bass as bass
import concourse.mybir as mybir
from concourse.bass_utils import run_bass_kernel_spmd

F32 = mybir.dt.float32
BF16 = mybir.dt.bfloat16
AF = mybir.ActivationFunctionType
ALU = mybir.AluOpType

D = 1024
CTX = 256
NE = 32
EPS = 1e-6
INW = 4624
ENGS = ("pe", "act", "dve", "pool", "sp")
SEM_LIMIT = 30000


class Res:
    __slots__ = ("name", "last_w", "readers", "dma_sem", "dma_cnt")

    def __init__(self, name):
        self.name = name
        self.last_w = None
        self.readers = []
        self.dma_sem = None
        self.dma_cnt = 0


class Op:
    __slots__ = ("eng", "fn", "deps", "idx", "signal", "sig", "dma_res", "dma_cnt", "known", "waits")

    def __init__(self, eng, fn):
        self.eng = eng
        self.fn = fn
        self.deps = []
        self.idx = -1
        self.signal = False
        self.sig = 0
        self.dma_res = None
        self.dma_cnt = 0
        self.known = None
        self.waits = []


class Sched:
    def __init__(self, nc):
        self.nc = nc
        self.ops = {e: [] for e in ENGS}
        self.all = []
        self.res = {}
        self.since_bar = []

    def R(self, name):
        r = self.res.get(name)
        if r is None:
            r = Res(name)
            self.res[name] = r
        return r

    def add(self, eng, fn, reads=(), writes=(), dma=None):
        op = Op(eng, fn)
        deps = {}
        for r in reads:
            if r.last_w is not None:
                deps[id(r.last_w)] = r.last_w
        for r in writes:
            if r.last_w is not None:
                deps[id(r.last_w)] = r.last_w
            for q in r.readers:
                deps[id(q)] = q
        for r in reads:
            r.readers.append(op)
        for r in writes:
            r.last_w = op
            r.readers = []
        deps.pop(id(op), None)
        op.deps = list(deps.values())
        if dma is not None:
            op.dma_res = dma
            dma.dma_cnt += 1
            op.dma_cnt = dma.dma_cnt
        op.idx = len(self.ops[eng])
        self.ops[eng].append(op)
        self.all.append(op)
        self.since_bar.append(op)
        return op

    def barrier(self):
        deps = {}
        for op in self.since_bar:
            if op.dma_res is not None:
                k = ("dma", op.dma_res.name)
            else:
                k = op.eng
            deps[k] = op
        dl = list(deps.values())
        self.since_bar = []
        for e in ENGS:
            op = Op(e, None)
            op.deps = list(dl)
            op.idx = len(self.ops[e])
            self.ops[e].append(op)
            self.all.append(op)
            self.since_bar.append(op)
        for r in self.res.values():
            r.last_w = None
            r.readers = []

    def finalize(self):
        known = {e: {} for e in ENGS}
        for op in self.all:
            kn = known[op.eng]
            waits = {}
            for d in op.deps:
                if d.dma_res is not None:
                    key = ("dma", d.dma_res.name)
                    val = d.dma_cnt
                else:
                    if d.eng == "pe" and op.eng == "pe":
                        continue
                    key = d.eng
                    val = d.idx + 1
                if kn.get(key, 0) >= val:
                    continue
                if waits.get(key, (0, None))[0] < val:
                    waits[key] = (val, d)
            op.waits = []
            if waits:
                kn = dict(kn)
                known[op.eng] = kn
            for key, (val, d) in waits.items():
                kn[key] = max(kn.get(key, 0), val)
                if d.known is not None:
                    for k2, v2 in d.known.items():
                        if kn.get(k2, 0) < v2:
                            kn[k2] = v2
                if d.dma_res is None:
                    d.signal = True
                op.waits.append(d)
            op.known = kn
        self.nsig = {}
        for e in ENGS:
            c = 0
            for op in self.ops[e]:
                if op.signal and op.dma_res is None:
                    c += 1
                    op.sig = c
            self.nsig[e] = c

    def emit(self, stack):
        nc = self.nc
        self.finalize()
        sems = {}

        def esem(e, epoch):
            k = (e, epoch)
            if k not in sems:
                sems[k] = stack.enter_context(nc.semaphore(f"s_{e}_{epoch}"))
            return sems[k]

        for e in ENGS:
            for ep in range((self.nsig[e] + SEM_LIMIT - 1) // SEM_LIMIT):
                esem(e, ep)
        for r in self.res.values():
            if r.dma_cnt > 0:
                r.dma_sem = stack.enter_context(nc.semaphore(f"d_{r.name}"))
        block = stack.enter_context(nc.Block())

        def run(eng_name, eng):
            for op in self.ops[eng_name]:
                for d in op.waits:
                    if d.dma_res is not None:
                        eng.wait_ge(d.dma_res.dma_sem, 16 * d.dma_cnt)
                    else:
                        s = d.sig - 1
                        eng.wait_ge(esem(d.eng, s // SEM_LIMIT), (s % SEM_LIMIT) + 1)
                if op.fn is None:
                    if op.signal:
                        s = op.sig - 1
                        eng.sem_inc(esem(op.eng, s // SEM_LIMIT), 1)
                    continue
                ins = op.fn(eng)
                if op.dma_res is not None:
                    ins.then_inc(op.dma_res.dma_sem, 16)
                elif op.signal:
                    s = op.sig - 1
                    ins.then_inc(esem(op.eng, s // SEM_LIMIT), 1)

        @block.tensor
        def _(eng):
            run("pe", eng)

        @block.scalar
        def _(eng):
            run("act", eng)

        @block.vector
        def _(eng):
            run("dve", eng)

        @block.gpsimd
        def _(eng):
            run("pool", eng)

        @block.sync
        def _(eng):
            run("sp", eng)


class Tile:
    __slots__ = ("ap", "r")

    def __init__(self, ap, r):
        self.ap = ap
        self.r = r

    def __getitem__(self, k):
        return self.ap[k]


def build(L, debug=False, ne_run=NE):
    NT = L // 512
    NCH = L // 128
    LT = CTX + L
    NGC = LT // 128
    NG = L // 1024
    nc = bass.Bass("TRN2", target_bir_lowering=False)

    def din(name, shape, dt=F32):
        return nc.dram_tensor(name, list(shape), dt, kind="ExternalInput").ap()

    x = din("x", [L, D]); ctx = din("ctx", [CTX, D]); cT_d = din("cT", [128, 8, 2])
    w_ada = din("w_ada", [D, 6 * D]); b_ada_c = din("b_ada_c", [128, 48])
    n1c_d = din("n1c", [128, 8]); n2c_d = din("n2c", [128, 8]); fnwb_d = din("fnwb", [128, D])
    w_in = din("w_in", [D, INW]); gbc_d = din("gbc", [16, 1]); cwc_d = din("cwc", [128, 8, 3]); cbc_d = din("cbc", [128, 8])
    w_pool = din("w_pool", [4, 128, 128]); psc_d = din("psc", [128, 4]); hnc_d = din("hnc", [128, 4])
    w_bp = din("w_bp", [512, D]); w_bm = din("w_bm", [512, D]); w_out = din("w_out", [D, D])
    w_router = din("w_router", [D, NE]); brb_d = din("brb", [128, NE])
    w1 = din("w1", [NE, D, 2 * D]); b1c_d = din("b1c", [128, NE, 16]); w2 = din("w2", [NE, D, D]); b2_d = din("b2", [NE, D])
    ident_d = din("ident", [128, 128]); maskf_d = din("maskf", [128, 128]); maskb_d = din("maskb", [128, 128])
    pmat_d = din("pmat", [128, 4, 128]); sel_d = din("sel", [16, 16, 128])
    out = nc.dram_tensor("out", [L, D], F32, kind="ExternalOutput").ap()

    def dscr(name, shape, dt):
        return nc.dram_tensor(name, list(shape), dt, kind="ExternalOutput" if debug else "Internal").ap()

    S_qk = dscr("S_qk", [1024, LT], BF16); S_v = dscr("S_v", [LT, 512], BF16); S_g = dscr("S_g", [16, LT], F32)
    S_o = dscr("S_o", [512, L], BF16); S_gb = dscr("S_gb", [1024, L], BF16); S_za = dscr("S_za", [1024, L], BF16)
    S_yb = dscr("S_yb", [512, L], BF16)
    S_gs = dscr("S_gs", [5, 16, LT], F32)

    S = Sched(nc)
    R = S.R
    with ExitStack() as st:
        ARW = 53000
        arena = st.enter_context(nc.sbuf_tensor("arena", [128, ARW], F32))
        state = {"off": 0, "gen": 0}

        def T(name, shape, dt=F32, parts=128, res=None):
            n = 1
            for s_ in shape:
                n *= s_
            words = (n + 1) // 2 if dt == BF16 else n
            off = state["off"]
            assert off + words <= ARW, (name, off, words)
            state["off"] = off + words
            ap = arena[0:parts, off:off + words]
            if dt == BF16:
                ap = ap.bitcast(BF16)
                if n % 2:
                    ap = ap[:, 0:n]
            if len(shape) == 2:
                ap = ap.rearrange("p (a b) -> p a b", a=shape[0])
            elif len(shape) == 3:
                ap = ap.rearrange("p (a b c) -> p a b c", a=shape[0], b=shape[1])
            return Tile(ap, R(res or f"{name}.{state['gen']}"))

        banks = [st.enter_context(nc.psum_tensor(f"bank{i}", [128, 512], F32)) for i in range(8)]
        bstate = {"i": 0}

        def bank():
            i = bstate["i"]
            bstate["i"] = (i + 1) % 8
            return Tile(banks[i][:], R(f"bank{i}"))

        def RS(ts):
            return [t.r if isinstance(t, Tile) else t for t in ts]

        def dma(q, o, i, reads=(), writes=(), sem=None):
            S.add(q, lambda e: e.dma_start(out=o, in_=i), RS(reads), RS(writes), dma=sem.r if isinstance(sem, Tile) else sem)

        def mm(o, l, r_, start, stop, reads, writes):
            S.add("pe", lambda e: e.matmul(o, lhsT=l, rhs=r_, start=start, stop=stop), RS(reads), RS(writes))

        def tr(o, i, idn, reads, writes):
            S.add("pe", lambda e: e.transpose(out=o, in_=i, identity=idn), RS(reads), RS(writes))

        def act(o, i, func, reads, writes, bias=None, scale=None, accum=None):
            kw = {}
            if bias is not None:
                kw["bias"] = bias
            if scale is not None:
                kw["scale"] = scale
            if accum is not None:
                kw["accum_out"] = accum
            S.add("act", lambda e: e.activation(out=o, in_=i, func=func, **kw), RS(reads), RS(writes))

        def ts(eng, o, i, s1, s2, op0, op1, reads, writes):
            if op1 is None:
                S.add(eng, lambda e: e.tensor_scalar(out=o, in0=i, scalar1=s1, scalar2=None, op0=op0), RS(reads), RS(writes))
            else:
                S.add(eng, lambda e: e.tensor_scalar(out=o, in0=i, scalar1=s1, scalar2=s2, op0=op0, op1=op1), RS(reads), RS(writes))

        def tt(eng, o, a, b, op, reads, writes):
            S.add(eng, lambda e: e.tensor_tensor(out=o, in0=a, in1=b, op=op), RS(reads), RS(writes))

        def stt(eng, o, a, sc, b, op0, op1, reads, writes):
            S.add(eng, lambda e: e.scalar_tensor_tensor(out=o, in0=a, scalar=sc, in1=b, op0=op0, op1=op1), RS(reads), RS(writes))

        def cp(eng, o, i, reads, writes):
            S.add(eng, lambda e: e.tensor_copy(out=o, in_=i), RS(reads), RS(writes))

        def ms(eng, o, v, writes):
            S.add(eng, lambda e: e.memset(o, v), (), RS(writes))

        def recip(o, i, reads, writes):
            S.add("dve", lambda e: e.reciprocal(out=o, in_=i), RS(reads), RS(writes))

        identf = T("identf", [128]); identb = T("identb", [128], BF16); onesf = T("onesf", [128])
        maskf = T("maskf", [128]); maskb = T("maskb", [128])
        modc = T("modc", [48, 2]); a1 = T("a1", [8]); ca1 = T("ca1", [8]); a2 = T("a2", [8])
        n1c = T("n1c", [8]); n2c = T("n2c", [8]); bac = T("bac", [48])
        gbc = T("gbc", [1], parts=16); cwc = T("cwc", [8, 3]); cbc = T("cbc", [8]); psc = T("psc", [4]); hnc = T("hnc", [4])
        ssq = T("ssq", [8]); std = T("std", [8]); rstd = T("rstd", [8])
        junkb = T("junkb", [D], BF16)
        PERSIST = state["off"]

        for t_, d_ in ((identf, ident_d), (maskf, maskf_d), (maskb, maskb_d), (n1c, n1c_d), (n2c, n2c_d), (bac, b_ada_c),
                       (cwc, cwc_d), (cbc, cbc_d), (psc, psc_d), (hnc, hnc_d)):
            dma("sp", t_.ap, d_, writes=[t_], sem=t_)
        dma("sp", gbc.ap, gbc_d, writes=[gbc], sem=gbc)
        cp("dve", identb.ap, identf.ap, [identf], [identb])
        ms("dve", onesf.ap, 1.0, [onesf])

        def reset_arena():
            S.barrier()
            state["off"] = PERSIST
            state["gen"] += 1

        cT = T("cT", [8, 2]); sg0 = T("sg0", [8, 2]); scT = T("scT", [8, 2], BF16)
        dma("sp", cT.ap, cT_d, writes=[cT], sem=cT)
        act(sg0.ap, cT.ap, AF.Sigmoid, [cT], [sg0])
        tt("dve", scT.ap, cT.ap, sg0.ap, ALU.mult, [cT, sg0], [scT])
        wa = [T(f"wa{i}", [8, 512], BF16) for i in range(2)]
        psA = bank()
        for nt in range(12):
            w_ = wa[nt % 2]
            dma("pool", w_.ap, w_ada[:, nt * 512:(nt + 1) * 512].rearrange("(kc p) n -> p kc n", p=128), writes=[w_], sem=w_)
            for cc in range(4):
                idx = nt * 4 + cc
                for kc in range(8):
                    mm(psA[:, idx * 2:idx * 2 + 2], w_[:, kc, cc * 128:(cc + 1) * 128], scT[:, kc, :], kc == 0, kc == 7, [w_, scT], [psA])
        tt("dve", modc.ap, psA[:, 0:96].rearrange("p (a b) -> p a b", b=2), bac.ap.unsqueeze(2).to_broadcast([128, 48, 2]), ALU.add, [psA, bac], [modc])
        stt("dve", a1.ap, modc[:, 8:16, 0], 1.0, n1c.ap, ALU.add, ALU.mult, [modc, n1c], [a1])
        stt("dve", ca1.ap, modc[:, 8:16, 1], 1.0, n1c.ap, ALU.add, ALU.mult, [modc, n1c], [ca1])
        stt("dve", a2.ap, modc[:, 32:40, 0], 1.0, n2c.ap, ALU.add, ALU.mult, [modc, n2c], [a2])
        reset_arena()
        win = T("win", [8, INW], BF16)
        for kc in range(8):
            dma("pool", win[:, kc, :], w_in[kc * 128:(kc + 1) * 128, :], writes=[win], sem=win)
        wbp = T("wbp", [4, D], BF16)
        dma("pool", wbp.ap, w_bp.rearrange("(g p) n -> p g n", p=128), writes=[wbp], sem=wbp)
        wpl = T("wpl", [4, 128], BF16)
        dma("pool", wpl.ap, w_pool.rearrange("g c d -> c g d"), writes=[wpl], sem=wpl)
        pmt = T("pmt", [4, 128], BF16)
        dma("pool", pmt.ap, pmat_d, writes=[pmt], sem=pmt)
        XT = [T(f"xt{i}", [4, D]) for i in range(2)]
        xn = T("xn", [4, D], BF16); hT = T("hT", [8, 512], BF16)
        sqk = T("sqk", [8, 512], BF16); so = T("so", [4, 512], BF16); sgb = T("sgb", [8, 512], BF16); gaT = T("gaT", [8, 512], BF16)
        sza = T("sza", [8, 512], BF16); sv = T("sv", [4, 512], BF16); ub = T("ub", [4, 512], BF16)
        plT = T("plT", [4, 512], BF16); yaT = T("yaT", [4, 512], BF16); sgt = T("sgt", [512], parts=16)

        def norm_transpose(xt, nsub, acol, shap, dst, n, xn):
            ms("dve", ssq[:, 0:nsub], 0.0, [ssq])
            for s_ in range(nsub):
                act(junkb.ap, xt[:, s_, :], AF.Square, [xt], [junkb, ssq], accum=ssq[:, s_:s_ + 1])
            act(std[:, 0:nsub], ssq[:, 0:nsub], AF.Sqrt, [ssq], [std], scale=1.0 / D, bias=EPS)
            recip(rstd[:, 0:nsub], std[:, 0:nsub], [std], [rstd])
            for s_ in range(nsub):
                act(xn[:, s_, :], xt[:, s_, :], AF.Copy, [xt, rstd], [xn], scale=rstd[:, s_:s_ + 1])
            for j in range(8):
                bk = bank()
                bb = bk.ap.bitcast(BF16)
                for s_ in range(nsub):
                    tr(bb[:, s_ * 128:(s_ + 1) * 128], xn[:, s_, j * 128:(j + 1) * 128], identb.ap, [xn, identb], [bk])
                ts("dve", dst[:, j, 0:n], bb[:, 0:n], acol[:, j:j + 1], shap(j), ALU.mult, ALU.add, [bk, a1, ca1, a2, modc], [dst])

        tiles = [(ctx, 0, 2, 0, True)] + [(x, i * 512, 4, CTX + i * 512, False) for i in range(NT)]
        for ti, (src, t0, nsub, gt0, is_ctx) in enumerate(tiles):
            n = nsub * 128
            xt = XT[ti % 2]
            dma("sp", xt[:, 0:nsub, :], src[t0:t0 + n, :].rearrange("(s p) d -> p s d", p=128), writes=[xt], sem=xt)
            col = 1 if is_ctx else 0
            norm_transpose(xt, nsub, ca1 if is_ctx else a1, lambda j, col=col: modc[:, j, col:col + 1], hT, n, xn)

            def fm(col0, m, evac):
                bk = bank()
                for kc in range(8):
                    mm(bk[0:m, 0:n], win[:, kc, col0:col0 + m], hT[:, kc, 0:n], kc == 0, kc == 7, [win, hT], [bk])
                evac(bk)

            for c8 in range(8):
                if is_ctx and c8 < 4:
                    continue
                fm(512 + c8 * 128, 128, lambda bk, c8=c8: act(sqk[:, c8, 0:n], bk[:, 0:n], AF.Copy, [bk], [sqk]))
            if is_ctx:
                dma("sp", S_qk[512:1024, gt0:gt0 + n].rearrange("(c p) t -> p c t", p=128), sqk[:, 4:8, 0:n], reads=[sqk, R("S_qk")], sem=sqk)
            else:
                dma("sp", S_qk[:, gt0:gt0 + n].rearrange("(c p) t -> p c t", p=128), sqk[:, :, 0:n], reads=[sqk, R("S_qk")], sem=sqk)
            fm(2560, 16, lambda bk: cp("dve", sgt[:, 0:n], bk[0:16, 0:n], [bk], [sgt]))
            dma("sp", S_g[:, gt0:gt0 + n], sgt[:, 0:n], reads=[sgt, R("S_g")], sem=sgt)
            for s_ in range(nsub):
                bk = bank()
                for kc in range(8):
                    mm(bk.ap, hT[:, kc, s_ * 128:(s_ + 1) * 128], win[:, kc, 1536:2048], kc == 0, kc == 7, [hT, win], [bk])
                act(sv[:, s_, :], bk.ap, AF.Copy, [bk], [sv])
                if not is_ctx:
                    bk = bank()
                    for kc in range(8):
                        mm(bk.ap, hT[:, kc, s_ * 128:(s_ + 1) * 128], win[:, kc, 0:512], kc == 0, kc == 7, [hT, win], [bk])
                    act(ub[:, s_, :], bk.ap, AF.Copy, [bk], [ub])
            dma("sp", S_v[gt0:gt0 + n, :].rearrange("(s p) d -> p s d", p=128), sv[:, 0:nsub, :], reads=[sv, R("S_v")], sem=sv)
            if is_ctx:
                continue
            for c4 in range(4):
                fm(2048 + c4 * 128, 128, lambda bk, c4=c4: act(so[:, c4, :], bk.ap, AF.Sigmoid, [bk], [so]))
            dma("sp", S_o[:, t0:t0 + n].rearrange("(c p) t -> p c t", p=128), so.ap, reads=[so, R("S_o")], sem=so)
            for c8 in range(8):
                fm(2576 + c8 * 128, 128, lambda bk, c8=c8: act(gaT[:, c8, :], bk.ap, AF.Sigmoid, [bk], [gaT]))
            for c8 in range(8):
                fm(3600 + c8 * 128, 128, lambda bk, c8=c8: act(sgb[:, c8, :], bk.ap, AF.Sigmoid, [bk], [sgb]))
            dma("sp", S_gb[:, t0:t0 + n].rearrange("(c p) t -> p c t", p=128), sgb.ap, reads=[sgb, R("S_gb")], sem=sgb)
            for s_ in range(4):
                bk = bank()
                for g in range(4):
                    mm(bk[:, g * 128:(g + 1) * 128], ub[:, s_, g * 128:(g + 1) * 128], pmt[:, g, :], True, True, [ub, pmt], [bk])
                cp("dve", plT[:, :, s_ * 128:(s_ + 1) * 128], bk.ap.rearrange("p (g t) -> p g t", g=4), [bk], [plT])
            for g in range(4):
                bk = bank()
                mm(bk.ap, wpl[:, g, :], plT[:, g, :], True, True, [wpl, plT], [bk])
                act(yaT[:, g, :], bk.ap, AF.Copy, [bk, psc], [yaT], scale=psc[:, g:g + 1])
            for dc in range(8):
                bk = bank()
                for g in range(4):
                    mm(bk.ap, wbp[:, g, dc * 128:(dc + 1) * 128], yaT[:, g, :], g == 0, g == 3, [wbp, yaT], [bk])
                tt("dve", sza[:, dc, :], bk.ap, gaT[:, dc, :], ALU.mult, [bk, gaT], [sza])
            dma("sp", S_za[:, t0:t0 + n].rearrange("(c p) t -> p c t", p=128), sza.ap, reads=[sza, R("S_za")], sem=sza)

        reset_arena()
        sel = T("sel", [16, 128], parts=16); nsel = T("nsel", [16, 128], parts=16)
        dma("sp", sel.ap, sel_d, writes=[sel], sem=sel)
        ts("dve", nsel.ap, sel.ap, -1.0, None, ALU.mult, None, [sel], [nsel])
        qp = {d_: T(f"qp{d_}", [LT], BF16) for d_ in "fb"}
        kp = {d_: T(f"kp{d_}", [LT], BF16) for d_ in "fb"}
        k2 = {d_: T(f"k2{d_}", [NGC, 128], BF16) for d_ in "fb"}
        vext = T("vext", [NGC, 129], BF16)
        hacc = T("hacc", [NCH, 128])
        EB = {d_: T(f"EB{d_}", [NGC]) for d_ in "fb"}
        raw = {w_: T(f"raw{w_}", [514], BF16) for w_ in "qk"}
        accb = T("accb", [512]); qc = T("qc", [512]); kc_ = T("kc", [512])
        Gt = T("Gt", [512], parts=16); Pt = T("Pt", [512], parts=16)
        F0 = T("F0", [512], parts=16); F1 = T("F1", [512], parts=16); B0 = T("B0", [512], parts=16); B1 = T("B1", [512], parts=16)
        Fe = T("Fe", [512], parts=16); Be = T("Be", [512], parts=16)
        EE = [T(f"EE{i}", [512]) for i in range(2)]; k2T = T("k2T", [512], BF16)
        erot = [0]

        def nextE():
            erot[0] += 1
            return EE[erot[0] % 2]
        Cst = {d_: T(f"C{d_}", [129]) for d_ in "fb"}
        Cbf = {d_: T(f"Cbf{d_}", [129], BF16) for d_ in "fb"}
        SM = [T(f"SM{i}", [128], BF16) for i in range(4)]
        dab = [T(f"dab{i}", [1]) for i in range(4)]
        rdn = [T(f"rdn{i}", [1]) for i in range(4)]
        ssh = T("ssh", [NCH]); sth = T("sth", [NCH]); rsh = T("rsh", [NCH])
        hn = [T(f"hn{i}", [4, 128], BF16) for i in range(1)]
        sgo = [T(f"sgo{i}", [512], BF16) for i in range(1)]
        ybt = [T(f"ybt{i}", [512], BF16) for i in range(1)]
        mask = {"f": maskf, "b": maskb}

        blocks = [(0, CTX, True)] + [(CTX + 512 * i, 512, False) for i in range(NT)]
        def gates_block(bix):
            a, n, is_ctx = blocks[bix]
            nch = n // 128
            dma("sp", Gt[:, 0:n], S_g[:, a:a + n], reads=[R("S_g")], writes=[Gt], sem=Gt)
            ts("dve", Gt[:, 0:n], Gt[:, 0:n], gbc[:, 0:1], None, ALU.add, None, [Gt, gbc], [Gt])
            act(Pt[:, 0:n], Gt[:, 0:n], AF.Exp, [Gt], [Pt], scale=-1.0)
            act(Pt[:, 0:n], Pt[:, 0:n], AF.Ln, [Pt], [Pt], bias=1.0)

            def v3(t_):
                return t_[:, 0:n].rearrange("p (c t) -> p c t", t=128)

            cur = Pt
            for si, k in enumerate((1, 2, 4, 8, 16, 32, 64)):
                nx = F0 if si % 2 == 0 else F1
                tt("dve", v3(nx)[:, :, k:], v3(cur)[:, :, k:], v3(cur)[:, :, :128 - k], ALU.add, [cur], [nx])
                cp("dve", v3(nx)[:, :, :k], v3(cur)[:, :, :k], [cur], [nx])
                cur = nx
            CSf = cur
            cur = Pt
            for si, k in enumerate((1, 2, 4, 8, 16, 32, 64)):
                nx = B0 if si % 2 == 0 else B1
                tt("pool", v3(nx)[:, :, :128 - k], v3(cur)[:, :, :128 - k], v3(cur)[:, :, k:], ALU.add, [cur], [nx])
                cp("pool", v3(nx)[:, :, 128 - k:], v3(cur)[:, :, 128 - k:], [cur], [nx])
                cur = nx
            CSb = cur
            cp("dve", v3(Fe), v3(CSf)[:, :, 127:128].to_broadcast([16, nch, 128]), [CSf], [Fe])
            cp("pool", v3(Be), v3(CSb)[:, :, 0:1].to_broadcast([16, nch, 128]), [CSb], [Be])
            for gi_, t_ in enumerate((Gt, CSf, CSb, Fe, Be)):
                dma("sp", S_gs[gi_, :, a:a + n], t_[:, 0:n], reads=[t_, R("S_gs")], sem=t_)

        for bix_ in range(len(blocks)):
            gates_block(bix_)
        S.add("sp", None, writes=[R("S_gs")])
        for h in range(4):
            dma("sp", vext[:, :, 0:128], S_v[:, h * 128:(h + 1) * 128].rearrange("(c p) d -> p c d", p=128), reads=[R("S_v")], writes=[vext], sem=vext)
            ms("dve", vext[:, :, 128:129], 1.0, [vext])
            def b1_block(bix):
                a, n, is_ctx = blocks[bix]
                nch = n // 128
                gc0 = a // 128
                s0 = a in (0, CTX)
                s1 = (a + n) in (CTX, LT)
                for wname in "qk":
                    if is_ctx and wname == "q":
                        continue
                    rw = raw[wname]
                    ci = h if wname == "q" else 4 + h
                    row0 = ci * 128
                    if s0:
                        ms("dve", rw[:, 0:1], 0.0, [rw])
                    if s1:
                        ms("dve", rw[:, n + 1:n + 2], 0.0, [rw])
                    lo = a if s0 else a - 1
                    hi = a + n if s1 else a + n + 1
                    dma("sp", rw[:, lo - (a - 1):hi - (a - 1)], S_qk[row0:row0 + 128, lo:hi], reads=[R("S_qk")], writes=[rw], sem=rw)
                    ts("dve", accb[:, 0:n], rw[:, 1:n + 1], cwc[:, ci, 1:2], cbc[:, ci:ci + 1], ALU.mult, ALU.add, [rw, cwc, cbc], [accb])
                    stt("dve", accb[:, 0:n], rw[:, 0:n], cwc[:, ci, 0:1], accb[:, 0:n], ALU.mult, ALU.add, [rw, cwc, accb], [accb])
                    stt("dve", accb[:, 0:n], rw[:, 2:n + 2], cwc[:, ci, 2:3], accb[:, 0:n], ALU.mult, ALU.add, [rw, cwc, accb], [accb])
                    dst = qc if wname == "q" else kc_
                    act(dst[:, 0:n], accb[:, 0:n], AF.Silu, [accb], [dst])
                def v3(t_):
                    return t_[:, 0:n].rearrange("p (c t) -> p c t", t=128)
                CSf, CSb = F0, B0
                for gi_, t_ in enumerate((Gt, CSf, CSb, Fe, Be)):
                    dma("sp", t_[:, 0:n], S_gs[gi_, :, a:a + n], reads=[R("S_gs")], writes=[t_], sem=t_)
                for d_ in "fb":
                    CS, CSe = (CSf, Fe) if d_ == "f" else (CSb, Be)
                    ir = (0 if d_ == "f" else 8) + h
                    fr = (4 if d_ == "f" else 12) + h
                    if not is_ctx:
                        bk = bank()
                        mm(bk[:, 0:n], nsel[:, fr, :], CS[:, 0:n], True, True, [nsel, CS], [bk])
                        EQ = nextE()
                        act(EQ[:, 0:n], bk[:, 0:n], AF.Exp, [bk], [EQ])
                        stt("dve", qp[d_][:, a:a + n], qc[:, 0:n], 128.0 ** -0.5, EQ[:, 0:n], ALU.mult, ALU.mult, [qc, EQ], [R(f'qp{d_}{bix}')])
                        bk = bank()
                        mm(bk[:, 0:n], sel[:, ir, :], Gt[:, 0:n], True, False, [sel, Gt], [bk])
                        mm(bk[:, 0:n], sel[:, fr, :], CS[:, 0:n], False, True, [sel, CS], [bk])
                        EK = nextE()
                        act(EK[:, 0:n], bk[:, 0:n], AF.Exp, [bk], [EK])
                        tt("dve", kp[d_][:, a:a + n], kc_[:, 0:n], EK[:, 0:n], ALU.mult, [kc_, EK], [R(f'kp{d_}{bix}')])
                    bk = bank()
                    mm(bk[:, 0:n], sel[:, ir, :], Gt[:, 0:n], True, False, [sel, Gt], [bk])
                    mm(bk[:, 0:n], sel[:, fr, :], CS[:, 0:n], False, False, [sel, CS], [bk])
                    mm(bk[:, 0:n], nsel[:, fr, :], CSe[:, 0:n], False, True, [nsel, CSe], [bk])
                    EK2 = nextE()
                    act(EK2[:, 0:n], bk[:, 0:n], AF.Exp, [bk], [EK2])
                    tt("dve", k2T[:, 0:n], kc_[:, 0:n], EK2[:, 0:n], ALU.mult, [kc_, EK2], [k2T])
                    bk = bank()
                    bb = bk.ap.bitcast(BF16)
                    for c in range(nch):
                        tr(bb[:, c * 128:(c + 1) * 128], k2T[:, c * 128:(c + 1) * 128], identb.ap, [k2T, identb], [bk])
                    act(k2[d_][:, gc0:gc0 + nch, :], bb[:, 0:n].rearrange("p (c t) -> p c t", t=128), AF.Copy, [bk], [R(f'k2{d_}{bix}')])
                    bk = bank()
                    ecol = v3(CS)[:, :, 127] if d_ == "f" else v3(CS)[:, :, 0]
                    mm(bk[:, 0:nch], nsel[:, fr, :], ecol, True, True, [nsel, CS], [bk])
                    act(EB[d_][:, gc0:gc0 + nch], bk[:, 0:nch], AF.Exp, [bk], [R(f'EB{d_}{bix}')])

            for d_ in "fb":
                ms("dve", Cst[d_].ap, 0.0, [Cst[d_]])

            def blk_of(gc):
                return 0 if gc < 2 else 1 + (gc - 2) // 4

            def state_update(d_, gc):
                bx = blk_of(gc)
                bk = bank()
                mm(bk[:, 0:129], k2[d_][:, gc, :], vext[:, gc, :], True, True, [R(f'k2{d_}{bx}'), vext], [bk])
                stt("dve", Cst[d_].ap, Cst[d_].ap, EB[d_][:, gc:gc + 1], bk[:, 0:129], ALU.mult, ALU.add, [Cst[d_], R(f'EB{d_}{bx}'), bk], [Cst[d_]])


            def chunk_step(d_, c):
                gc = 2 + c
                tk = slice(gc * 128, (gc + 1) * 128)
                i_ = rot[0] % 4
                rot[0] += 1
                b1_ = bank()
                bx = blk_of(gc)
                mm(b1_[:, 0:128], kp[d_][:, tk], qp[d_][:, tk], True, True, [R(f'kp{d_}{bx}'), R(f'qp{d_}{bx}')], [b1_])
                tt("dve", SM[i_].ap, b1_[:, 0:128], mask[d_].ap, ALU.mult, [b1_, mask[d_]], [SM[i_]])
                b2_ = bank()
                mm(b2_[:, 0:129], SM[i_].ap, vext[:, gc, :], True, False, [SM[i_], vext], [b2_])
                mm(b2_[:, 0:129], qp[d_][:, tk], Cbf[d_].ap, False, True, [R(f'qp{d_}{bx}'), Cbf[d_]], [b2_])
                act(dab[i_].ap, b2_[:, 128:129], AF.Abs, [b2_], [dab[i_]])
                ts("dve", dab[i_].ap, dab[i_].ap, 1.0, None, ALU.max, None, [dab[i_]], [dab[i_]])
                recip(rdn[i_].ap, dab[i_].ap, [dab[i_]], [rdn[i_]])
                hr = R(f"hacc{c}")
                if c not in seen:
                    seen.add(c)
                    act(hacc[:, c, :], b2_[:, 0:128], AF.Copy, [b2_, rdn[i_]], [hr], scale=rdn[i_][:, 0:1])
                else:
                    stt("dve", hacc[:, c, :], b2_[:, 0:128], rdn[i_][:, 0:1], hacc[:, c, :], ALU.mult, ALU.add, [b2_, rdn[i_], hr], [hr])
                state_update(d_, gc)
                act(Cbf[d_].ap, Cst[d_].ap, AF.Copy, [Cst[d_]], [Cbf[d_]])

            def b3_block(bi):
                hn_ = hn[0]; sg_ = sgo[0]; yb_ = ybt[0]
                c0 = bi * 4
                hr4 = [R(f"hacc{c}") for c in range(c0, c0 + 4)]
                ms("dve", ssh[:, c0:c0 + 4], 0.0, [ssh])
                for c in range(c0, c0 + 4):
                    act(junkb[:, 0:128], hacc[:, c, :], AF.Square, [R(f"hacc{c}")], [junkb, ssh], accum=ssh[:, c:c + 1])
                act(sth[:, c0:c0 + 4], ssh[:, c0:c0 + 4], AF.Sqrt, [ssh], [sth], scale=1.0 / 128, bias=EPS)
                recip(rsh[:, c0:c0 + 4], sth[:, c0:c0 + 4], [sth], [rsh])
                dma("sp", sg_.ap, S_o[h * 128:(h + 1) * 128, bi * 512:(bi + 1) * 512], reads=[R("S_o")], writes=[sg_], sem=sg_)
                tt("dve", hn_.ap, hacc[:, c0:c0 + 4, :], rsh[:, c0:c0 + 4].unsqueeze(2).to_broadcast([128, 4, 128]), ALU.mult, hr4 + [rsh.r], [hn_])
                bk = bank()
                bb = bk.ap.bitcast(BF16)
                for c in range(4):
                    tr(bb[:, c * 128:(c + 1) * 128], hn_[:, c, :], identb.ap, [hn_, identb], [bk])
                stt("dve", yb_.ap, bb[:, 0:512], hnc[:, h:h + 1], sg_.ap, ALU.mult, ALU.mult, [bk, hnc, sg_], [yb_])
                dma("sp", S_yb[h * 128:(h + 1) * 128, bi * 512:(bi + 1) * 512], yb_.ap, reads=[yb_, R("S_yb")], sem=yb_)

            b1_block(0)
            done_b1 = set()
            for bx_ in (1, NT):
                if bx_ not in done_b1:
                    b1_block(bx_); done_b1.add(bx_)
            for gc in (0, 1):
                state_update("f", gc)
            for gc in (1, 0):
                state_update("b", gc)
            for d_ in "fb":
                act(Cbf[d_].ap, Cst[d_].ap, AF.Copy, [Cst[d_]], [Cbf[d_]])
            seen = set()
            rot = [0]
            for i in range(NT):
                for cc_ in range(4):
                    chunk_step("f", i * 4 + cc_)
                    chunk_step("b", (NT - 1 - i) * 4 + 3 - cc_)
                for bx_ in (i + 2, NT - 1 - i):
                    if 1 <= bx_ <= NT and bx_ not in done_b1:
                        b1_block(bx_); done_b1.add(bx_)
                if 2 * i + 1 >= NT:
                    for bq in sorted({i, NT - 1 - i}):
                        b3_block(bq)
            assert len(done_b1) == NT

        reset_arena()
        g1b = T("g1b", [D]); g2b = T("g2b", [D]); fnwb = T("fnwb", [D]); brb = T("brb", [NE])
        dma("sp", fnwb.ap, fnwb_d, writes=[fnwb], sem=fnwb)
        dma("sp", brb.ap, brb_d, writes=[brb], sem=brb)
        wbm = T("wbm", [4, D], BF16); wout = T("wout", [8, D], BF16); wrt = T("wrt", [8, NE], BF16)
        dma("pool", wbm.ap, w_bm.rearrange("(g p) n -> p g n", p=128), writes=[wbm], sem=wbm)
        dma("pool", wout.ap, w_out.rearrange("(g p) n -> p g n", p=128), writes=[wout], sem=wout)
        dma("pool", wrt.ap, w_router.rearrange("(g p) n -> p g n", p=128), writes=[wrt], sem=wrt)
        b1c = T("b1c", [NE, 16]); b1p = T("b1p", [NE, 16])
        dma("sp", b1c.ap, b1c_d, writes=[b1c], sem=b1c)
        ts("dve", b1p.ap, b1c.ap, 1.0, None, ALU.add, None, [b1c], [b1p])
        b2g = T("b2g", [D], BF16, parts=32)
        x1 = T("x1", [8, D]); h2T = T("h2T", [8, 1024], BF16)
        wts = T("wts", [8, NE]); wtT = T("wtT", [8, 128], BF16, parts=32)
        lg = T("lg", [NE]); t8 = T("t8", [8]); nm1 = T("nm1", [1]); mk = T("mk", [NE]); ex = T("ex", [NE]); wsum = T("wsum", [1]); rws = T("rws", [1])
        ring = [T(f"ring{i}", [8, D], BF16) for i in range(4)]
        ring_i = [0]

        def next_unit():
            u = ring[ring_i[0] % 4]
            ring_i[0] += 1
            return u
        c_off = state["off"]
        b2f = T("b2f", [D], parts=32)
        dma("sp", b2f.ap, b2_d, writes=[b2f], sem=b2f)
        dg = [T(f"dg{i}", [128]) for i in range(2)]
        for gi, (gt, base) in enumerate(((g1b, 16), (g2b, 40))):
            for hf in range(2):
                bk = bank()
                for jj in range(4):
                    j = hf * 4 + jj
                    d_ = dg[j % 2]
                    ts("dve", d_.ap, identf.ap, modc[:, base + j, 0:1], None, ALU.mult, None, [identf, modc], [d_])
                    mm(bk[:, jj * 128:(jj + 1) * 128], onesf.ap, d_.ap, True, True, [onesf, d_], [bk])
                cp("dve", gt[:, hf * 512:(hf + 1) * 512], bk.ap, [bk], [gt])

        tt("dve", b2g.ap, b2f.ap, g2b[0:32, :], ALU.mult, [b2f, g2b], [b2g])
        state["off"] = c_off
        ybT = T("ybT", [4, 512], BF16, res="cA"); gbT = T("gbT", [8, 512], BF16, res="cB"); zaT = T("zaT", [8, 512], BF16, res="cC")
        yT = T("yT", [8, 512], BF16, res="cD"); tmpA = T("tmpA", [512], res="cE"); tmpB = T("tmpB", [512], res="cF"); xn2 = T("xn2", [4, D], BF16)
        c_end = state["off"]
        state["off"] = c_off
        actT = [T(f"actT{i}", [8, 512], BF16, res=r_) for i, r_ in enumerate(("cA", "cB"))]
        gcb = [T(f"gcb{i}", [512], res=r_) for i, r_ in enumerate(("cC", "cC"))]
        sgb_ = [T(f"sgx{i}", [512], res=r_) for i, r_ in enumerate(("cD", "cD"))]
        ucb = [T(f"ucb{i}", [512], res=r_) for i, r_ in enumerate(("cE", "cE"))]
        t1b = [T(f"t1b{i}", [512], res=r_) for i, r_ in enumerate(("cF", "cF"))]
        state["off"] = max(state["off"], c_end)
        for lst, nm in ((actT, "actT"), (gcb, "gcb"), (sgb_, "sgx"), (ucb, "ucb"), (t1b, "t1b")):
            for i, t_ in enumerate(lst):
                t_.r = R(f"{nm}{i}")
        for t_, nm in ((ybT, "ybT"), (gbT, "gbT"), (zaT, "zaT"), (yT, "yT"), (tmpA, "tmpA"), (tmpB, "tmpB")):
            t_.r = R(nm)

        def load_w1(e, half):
            u = next_unit()
            dma("pool", u.ap, w1[e, :, half * D:(half + 1) * D].rearrange("(kc p) n -> p kc n", p=128), writes=[u], sem=u)
            return u

        def load_w2(e):
            u = next_unit()
            dma("pool", u.ap, w2[e].rearrange("(kc p) n -> p kc n", p=128), writes=[u], sem=u)
            tt("pool", u.ap, u.ap, g2b.ap.unsqueeze(1).to_broadcast([128, 8, D]), ALU.mult, [u, g2b], [u])
            return u

        for g in range(NG):
            S.barrier()
            T0 = g * 1024
            for tti in range(2):
                t0 = T0 + tti * 512
                dma("sp", x1[:, tti * 4:(tti + 1) * 4, :], x[t0:t0 + 512, :].rearrange("(s p) d -> p s d", p=128), writes=[x1], sem=x1)
                dma("sp", ybT.ap, S_yb[:, t0:t0 + 512].rearrange("(c p) t -> p c t", p=128), reads=[R("S_yb")], writes=[ybT], sem=ybT)
                dma("sp", gbT.ap, S_gb[:, t0:t0 + 512].rearrange("(c p) t -> p c t", p=128), reads=[R("S_gb")], writes=[gbT], sem=gbT)
                dma("sp", zaT.ap, S_za[:, t0:t0 + 512].rearrange("(c p) t -> p c t", p=128), reads=[R("S_za")], writes=[zaT], sem=zaT)
                for dc in range(8):
                    bk = bank()
                    for kc in range(4):
                        mm(bk.ap, wbm[:, kc, dc * 128:(dc + 1) * 128], ybT[:, kc, :], kc == 0, kc == 3, [wbm, ybT], [bk])
                    tt("dve", tmpA.ap, bk.ap, gbT[:, dc, :], ALU.mult, [bk, gbT], [tmpA])
                    tt("dve", yT[:, dc, :], tmpA.ap, zaT[:, dc, :], ALU.add, [tmpA, zaT], [yT])
                for s_ in range(4):
                    for hf in range(2):
                        bk = bank()
                        for kc in range(8):
                            mm(bk.ap, yT[:, kc, s_ * 128:(s_ + 1) * 128], wout[:, kc, hf * 512:(hf + 1) * 512], kc == 0, kc == 7, [yT, wout], [bk])
                        xs = x1[:, tti * 4 + s_, hf * 512:(hf + 1) * 512]
                        tt("dve", tmpB.ap, bk.ap, g1b[:, hf * 512:(hf + 1) * 512], ALU.mult, [bk, g1b], [tmpB])
                        tt("dve", xs, xs, tmpB.ap, ALU.add, [x1, tmpB], [x1])
                xv = Tile(x1[:, tti * 4:(tti + 1) * 4, :], x1.r)
                dst = Tile(h2T[:, :, tti * 512:(tti + 1) * 512], h2T.r)
                norm_transpose(xv, 4, a2, lambda j: modc[:, 24 + j, 0:1], dst, 512, xn2)
                for s_ in range(4):
                    sg_ = tti * 4 + s_
                    bk = bank()
                    for kc in range(8):
                        mm(bk[:, 0:NE], h2T[:, kc, sg_ * 128:(sg_ + 1) * 128], wrt[:, kc, :], kc == 0, kc == 7, [h2T, wrt], [bk])
                    tt("dve", lg.ap, bk[:, 0:NE], brb.ap, ALU.add, [bk, brb], [lg])
                    S.add("dve", lambda e: e.max(out=t8.ap, in_=lg.ap), RS([lg]), RS([t8]))
                    ts("dve", nm1.ap, t8[:, 0:1], -1.0, None, ALU.mult, None, [t8], [nm1])
                    ts("dve", mk.ap, lg.ap, t8[:, 3:4], 1e30, ALU.subtract, ALU.mult, [lg, t8], [mk])
                    ts("dve", mk.ap, mk.ap, 1.0, 0.0, ALU.add, ALU.max, [mk], [mk])
                    ts("dve", mk.ap, mk.ap, 1.0, None, ALU.min, None, [mk], [mk])
                    act(ex.ap, lg.ap, AF.Exp, [lg, nm1], [ex], bias=nm1[:, 0:1])
                    tt("dve", ex.ap, ex.ap, mk.ap, ALU.mult, [ex, mk], [ex])
                    S.add("dve", lambda e: e.reduce_sum(out=wsum.ap, in_=ex.ap, axis=mybir.AxisListType.X), RS([ex]), RS([wsum]))
                    recip(rws.ap, wsum.ap, [wsum], [rws])
                    ts("dve", wts[:, sg_, :], ex.ap, rws[:, 0:1], None, ALU.mult, None, [ex, rws], [wts])
                    bk = bank()
                    S.add("pe", lambda e, bk=bk, sg_=sg_: e.transpose(out=bk[0:NE, 0:128], in_=wts[:, sg_, :], identity=identf.ap), RS([wts, identf]), RS([bk]))
                    cp("dve", wtT[:, sg_, :], bk[0:NE, 0:128], [bk], [wtT])
                    for hf in range(2):
                        bk = bank()
                        mm(bk.ap, wtT[:, sg_, :], b2g[:, hf * 512:(hf + 1) * 512], True, True, [wtT, b2g], [bk])
                        xs = x1[:, sg_, hf * 512:(hf + 1) * 512]
                        tt("dve", xs, xs, bk.ap, ALU.add, [x1, bk], [x1])
            S.barrier()
            nxt = [load_w1(0, 0), load_w1(0, 1), load_w2(0)] if ne_run > 0 else None
            for e in range(ne_run):
                ua, ub_, uc = nxt
                if e + 1 < ne_run:
                    nxt = [load_w1(e + 1, 0)]
                for tti in range(2):
                    aT = actT[tti]
                    for fp in range(8):
                        i_ = fp % 2
                        bg = bank()
                        for kc in range(8):
                            mm(bg.ap, ua[:, kc, fp * 128:(fp + 1) * 128], h2T[:, kc, tti * 512:(tti + 1) * 512], kc == 0, kc == 7, [ua, h2T], [bg])
                        bu = bank()
                        for kc in range(8):
                            mm(bu.ap, ub_[:, kc, fp * 128:(fp + 1) * 128], h2T[:, kc, tti * 512:(tti + 1) * 512], kc == 0, kc == 7, [ub_, h2T], [bu])
                        ts("dve", gcb[i_].ap, bg.ap, b1c[:, e, fp:fp + 1], 7.0, ALU.add, ALU.min, [bg, b1c], [gcb[i_]])
                        act(sgb_[i_].ap, gcb[i_].ap, AF.Sigmoid, [gcb[i_]], [sgb_[i_]], scale=1.702)
                        ts("dve", ucb[i_].ap, bu.ap, b1p[:, e, 8 + fp:9 + fp], 8.0, ALU.add, ALU.min, [bu, b1p], [ucb[i_]])
                        tt("dve", t1b[i_].ap, gcb[i_].ap, sgb_[i_].ap, ALU.mult, [gcb[i_], sgb_[i_]], [t1b[i_]])
                        stt("dve", aT[:, fp, :], ucb[i_].ap, -6.0, t1b[i_].ap, ALU.max, ALU.mult, [ucb[i_], t1b[i_]], [aT])
                if e + 1 < ne_run:
                    nxt.append(load_w1(e + 1, 1))
                    nxt.append(load_w2(e + 1))
                for tti in range(2):
                    aT = actT[tti]
                    for s_ in range(4):
                        sg_ = tti * 4 + s_
                        for hf in range(2):
                            bk = bank()
                            for fc in range(8):
                                mm(bk.ap, aT[:, fc, s_ * 128:(s_ + 1) * 128], uc[:, fc, hf * 512:(hf + 1) * 512], fc == 0, fc == 7, [aT, uc], [bk])
                            xs = x1[:, sg_, hf * 512:(hf + 1) * 512]
                            stt("dve", xs, bk.ap, wts[:, sg_, e:e + 1], xs, ALU.mult, ALU.add, [bk, wts, x1], [x1])
            ms("dve", ssq.ap, 0.0, [ssq])
            for s_ in range(8):
                act(junkb.ap, x1[:, s_, :], AF.Square, [x1], [junkb, ssq], accum=ssq[:, s_:s_ + 1])
            act(std.ap, ssq.ap, AF.Sqrt, [ssq], [std], scale=1.0 / D, bias=EPS)
            recip(rstd.ap, std.ap, [std], [rstd])
            for s_ in range(8):
                stt("dve", x1[:, s_, :], x1[:, s_, :], rstd[:, s_:s_ + 1], fnwb.ap, ALU.mult, ALU.mult, [x1, rstd, fnwb], [x1])
            dma("sp", out[T0:T0 + 1024, :].rearrange("(s p) d -> p s d", p=128), x1.ap, reads=[x1, R("out")], sem=x1)
        S.add("sp", None, writes=[R("out")])
        S.emit(st)
    return nc


def _consts():
    ident = np.eye(128, dtype=np.float32)
    s_ = np.arange(128)
    maskf = (s_[:, None] <= s_[None, :]).astype(np.float32)
    maskb = (s_[:, None] >= s_[None, :]).astype(np.float32)
    pm = np.zeros((128, 4, 128), np.float32)
    pos = np.arange(64)
    for g, win in enumerate((2, 4, 8, 16)):
        lo = np.clip(pos - win // 2, 0, 64)
        hi = np.clip(pos + win // 2, 0, 64)
        P = np.zeros((64, 64), np.float32)
        for p in range(64):
            P[lo[p]:hi[p], p] = 1.0 / float(hi[p] - lo[p])
            P[p, p] -= 1.0
        pm[0:64, g, 0:64] = P
        pm[64:128, g, 64:128] = P
    sel = np.zeros((16, 16, 128), np.float32)
    for r in range(16):
        sel[r, r, :] = 1.0
    return ident, maskf, maskb, pm, sel


def _col(v, nchunk):
    return np.ascontiguousarray(np.asarray(v, np.float32).reshape(nchunk, 128).T)


def make_in_maps(inp, L, nb):
    f = lambda a: np.ascontiguousarray(np.asarray(a, dtype=np.float32))
    ident, maskf, maskb, pm, sel = _consts()
    shared = {
        "w_ada": f(inp["w_ada"][0]), "b_ada_c": _col(inp["b_ada"][0], 48),
        "n1c": _col(inp["norm1_w"][0], 8), "n2c": _col(inp["norm2_w"][0], 8),
        "fnwb": f(np.broadcast_to(np.asarray(inp["final_norm_w"], np.float32)[None, :], (128, D))),
        "w_in": f(inp["w_in"][0]), "gbc": f(np.asarray(inp["gate_b"][0]).reshape(16, 1)),
        "cwc": f(np.asarray(inp["conv_w"][0], np.float32).reshape(3, 8, 128).transpose(2, 1, 0)),
        "cbc": _col(inp["conv_b"][0], 8),
        "w_pool": f(inp["w_pool"][0]), "psc": _col(inp["pool_scale"][0], 4), "hnc": _col(inp["hnorm_w"][0], 4),
        "w_bp": f(inp["w_bp"][0]), "w_bm": f(inp["w_bm"][0]), "w_out": f(inp["w_out"][0]),
        "w_router": f(inp["w_router"][0]),
        "brb": f(np.broadcast_to(np.asarray(inp["b_router"][0], np.float32)[None, :], (128, NE))),
        "w1": f(inp["w1"][0]), "b1c": f(np.asarray(inp["b1"][0], np.float32).reshape(NE, 16, 128).transpose(2, 0, 1)),
        "w2": f(inp["w2"][0]), "b2": f(inp["b2"][0]),
        "ident": ident, "maskf": maskf, "maskb": maskb, "pmat": pm, "sel": sel,
    }
    maps = []
    cc = np.asarray(inp["c_ctx"], np.float32)
    for b in range(nb):
        m = dict(shared)
        m["x"] = f(inp["x"][b])
        m["ctx"] = f(inp["ctx"][b])
        cb = np.asarray(inp["c"][b], np.float32)
        m["cT"] = np.ascontiguousarray(np.stack([cb.reshape(8, 128).T, cc.reshape(8, 128).T], axis=-1))
        maps.append(m)
    return maps


def kernel(**inputs):
    xs = np.asarray(inputs["x"])
    B, L, _ = xs.shape
    nc = build(L)
    maps = make_in_maps(inputs, L, B)
    res = run_bass_kernel_spmd(nc, maps, core_ids=list(range(B)))
    return np.stack([np.asarray(r["out"], dtype=np.float32) for r in res.results], axis=0)
```
